# Optimizing a Trainium2 kernel written in Bass

```python
import math
import jax, jax.numpy as jnp
from jax import lax
import numpy as np

D_MODEL = 1024
BATCH = 4
SEQ = 8192
DEPTH = 2

D_MIX = 2 * D_MODEL
GROUP_W = D_MIX // 4
CHUNK = 128
Q_BLOCK = 128
ROPE_THETA = 10000.0
EPS = 1e-6
NEG_INF = -1e30

RET_HEADS = 4
RET_DK = 64
RET_DV = GROUP_W // RET_HEADS
DIFF_HEADS = 4
DIFF_DK = 64
DIFF_DV = GROUP_W // DIFF_HEADS
SSD_HEAD_DIM = 64
SSD_HEADS = GROUP_W // SSD_HEAD_DIM
SSD_GROUPS = 2
SSD_STATE = 128
SSD_CONV = 4
SSD_XBC = GROUP_W + 2 * SSD_GROUPS * SSD_STATE
MLSTM_HEADS = 4
MLSTM_DH = GROUP_W // MLSTM_HEADS
MLSTM_CONV = 4

RET_COLS = 2 * RET_HEADS * RET_DK + 2 * GROUP_W
DIFF_COLS = 4 * GROUP_W
SSD_COLS = SSD_XBC + SSD_HEADS + GROUP_W
MLSTM_COLS = 2 * GROUP_W + GROUP_W + GROUP_W + 2 * MLSTM_HEADS + GROUP_W
IN_COLS = RET_COLS + DIFF_COLS + SSD_COLS + MLSTM_COLS

kernel_name = 'hymba_style_retention_diffattn_ssd_mlstm'


def split_cols(a, sizes):
    offs = [int(o) for o in np.cumsum(sizes)[:-1]]
    return jnp.split(a, offs, axis=-1)


def _rms(x):
    return x * lax.rsqrt(jnp.mean(jnp.square(x), axis=-1, keepdims=True) + EPS)


def _layernorm(x):
    x = x - jnp.mean(x, axis=-1, keepdims=True)
    return _rms(x)


def rmsnorm(x, w):
    return _rms(x.astype(jnp.float32)) * w.astype(jnp.float32)


def rope_tables(seq, dim):
    inv = ROPE_THETA ** (-jnp.arange(0, dim, 2, dtype=jnp.float32) / dim)
    ang = jnp.arange(seq, dtype=jnp.float32)[:, None] * inv[None, :]
    return jnp.cos(ang), jnp.sin(ang)


def apply_rope(x, cos, sin):
    half = x.shape[-1] // 2
    shp = (cos.shape[0],) + (1,) * (x.ndim - 3) + (half,)
    c, s = cos.reshape(shp), sin.reshape(shp)
    x1, x2 = x[..., :half], x[..., half:]
    return jnp.concatenate([x1 * c - x2 * s, x2 * c + x1 * s], axis=-1)


def causal_conv(x, w, b):
    k, c = w.shape
    out = lax.conv_general_dilated(x, w.astype(x.dtype)[:, None, :], window_strides=(1,),
                                   padding=[(k - 1, 0)], dimension_numbers=('NWC', 'WIO', 'NWC'),
                                   feature_group_count=c)
    return out + b.astype(x.dtype)


def retention_mixer(q, k, v, cos, sin):
    bsz, seq, nh, _ = q.shape
    nc = seq // CHUNK
    q = apply_rope(q, cos, sin)
    k = apply_rope(k, cos, sin) * (RET_DK ** -0.5)
    log_g = jnp.log(1.0 - jnp.exp2(-5.0 - jnp.arange(nh, dtype=jnp.float32)))
    qc = q.reshape(bsz, nc, CHUNK, nh, RET_DK)
    kc = k.reshape(bsz, nc, CHUNK, nh, RET_DK)
    vc = v.reshape(bsz, nc, CHUNK, nh, RET_DV)
    pos = jnp.arange(CHUNK, dtype=jnp.float32)
    rel = pos[:, None] - pos[None, :]
    decay = jnp.where(rel >= 0, jnp.exp(log_g[:, None, None] * jnp.maximum(rel, 0.0)), 0.0)
    scores = jnp.einsum('bclhd,bcshd->bchls', qc, kc) * decay
    intra = jnp.einsum('bchls,bcshe->bclhe', scores, vc)
    k_w = jnp.exp(log_g[None, :] * (CHUNK - 1.0 - pos)[:, None])
    chunk_kv = jnp.einsum('bcshd,sh,bcshe->cbhde', kc, k_w, vc)
    chunk_decay = jnp.exp(log_g * CHUNK)[None, :, None, None]

    def step(state, kv):
        return state * chunk_decay + kv, state

    _, prev = lax.scan(step, jnp.zeros((bsz, nh, RET_DK, RET_DV), jnp.float32), chunk_kv)
    q_w = jnp.exp(log_g[None, :] * (pos + 1.0)[:, None])
    inter = jnp.einsum('bclhd,lh,cbhde->bclhe', qc, q_w, prev)
    out = _rms(intra + inter)
    return out.reshape(bsz, seq, nh * RET_DV)


def diff_attention_mixer(q, k, v, lam, lam_init, norm_w, cos, sin):
    bsz, seq, nh = q.shape[:3]
    nb = seq // Q_BLOCK
    q = apply_rope(q, cos, sin) * (DIFF_DK ** -0.5)
    k = apply_rope(k, cos, sin)
    qb = jnp.moveaxis(q.reshape(bsz, nb, Q_BLOCK, nh, 2, DIFF_DK), 1, 0)
    starts = jnp.arange(nb, dtype=jnp.int32) * Q_BLOCK
    kpos = jnp.arange(seq, dtype=jnp.int32)

    def block(args):
        qi, start = args
        s = jnp.einsum('bqhtd,bkhtd->bhtqk', qi, k)
        mask = kpos[None, :] <= (start + jnp.arange(Q_BLOCK, dtype=jnp.int32))[:, None]
        p = jax.nn.softmax(jnp.where(mask, s, NEG_INF), axis=-1)
        a = p[:, :, 0] - lam * p[:, :, 1]
        return jnp.einsum('bhqk,bkhe->bqhe', a, v)

    out = lax.map(block, (qb, starts))
    out = jnp.moveaxis(out, 0, 1).reshape(bsz, seq, nh, DIFF_DV)
    out = _rms(out) * norm_w * (1.0 - lam_init)
    return out.reshape(bsz, seq, nh * DIFF_DV)


def ssd_mixer(xbc, dt_raw, conv_w, conv_b, dt_bias, a_log, d_skip):
    bsz, seq, _ = xbc.shape
    nc = seq // CHUNK
    hg = SSD_HEADS // SSD_GROUPS
    xbc = jax.nn.silu(causal_conv(xbc, conv_w, conv_b))
    xs, bm, cm = split_cols(xbc, (GROUP_W, SSD_GROUPS * SSD_STATE, SSD_GROUPS * SSD_STATE))
    xs = xs.reshape(bsz, nc, CHUNK, SSD_GROUPS, hg, SSD_HEAD_DIM)
    bm = bm.reshape(bsz, nc, CHUNK, SSD_GROUPS, SSD_STATE)
    cm = cm.reshape(bsz, nc, CHUNK, SSD_GROUPS, SSD_STATE)
    dt = jax.nn.softplus(dt_raw + dt_bias).reshape(bsz, nc, CHUNK, SSD_GROUPS, hg)
    a = -jnp.exp(a_log).reshape(SSD_GROUPS, hg)
    a_cs = jnp.cumsum(jnp.moveaxis(dt * a, 2, -1), axis=-1)
    xdt = xs * dt[..., None]
    causal = jnp.tril(jnp.ones((CHUNK, CHUNK), bool))
    seg = a_cs[..., :, None] - a_cs[..., None, :]
    lmat = jnp.exp(jnp.where(causal, seg, -jnp.inf))
    cb = jnp.einsum('bclgn,bcsgn->bcgls', cm, bm)
    y_diag = jnp.einsum('bcgls,bcgrls,bcsgrp->bclgrp', cb, lmat, xdt)
    decay_states = jnp.exp(a_cs[..., -1:] - a_cs)
    states = jnp.einsum('bcsgn,bcgrs,bcsgrp->cbgrpn', bm, decay_states, xdt)
    chunk_decay = jnp.moveaxis(jnp.exp(a_cs[..., -1]), 1, 0)

    def step(h, inp):
        st, dec = inp
        return h * dec[..., None, None] + st, h

    _, prev = lax.scan(step, jnp.zeros(states.shape[1:], jnp.float32), (states, chunk_decay))
    y_off = jnp.einsum('bclgn,cbgrpn,bcgrl->bclgrp', cm, prev, jnp.exp(a_cs))
    y = y_diag + y_off + xs * d_skip.reshape(SSD_GROUPS, hg)[:, :, None]
    return y.reshape(bsz, seq, GROUP_W)


def mlstm_mixer(qk, v, o_raw, gate_raw, conv_w, conv_b, gate_b, norm_w):
    bsz, seq, _ = v.shape
    nh, dh = MLSTM_HEADS, MLSTM_DH
    nc = seq // CHUNK
    qk = jax.nn.silu(causal_conv(qk, conv_w, conv_b))
    q, k = jnp.split(qk, 2, axis=-1)

    def to_chunks(t):
        return t.reshape(bsz, nc, CHUNK, nh, dh).transpose(1, 0, 3, 2, 4)

    def gate_chunks(t):
        return t.reshape(bsz, nc, CHUNK, nh).transpose(1, 0, 3, 2)

    qc, kc, vc = to_chunks(q), to_chunks(k * (dh ** -0.5)), to_chunks(v)
    g = gate_raw + gate_b
    ic = gate_chunks(g[..., :nh])
    fc = gate_chunks(jax.nn.log_sigmoid(g[..., nh:]))
    causal = jnp.tril(jnp.ones((CHUNK, CHUNK), bool))

    def step(carry, inp):
        c_mat, n_vec, m = carry
        qj, kj, vj, ij, fj = inp
        b = jnp.cumsum(fj, axis=-1)
        d_log = jnp.where(causal, b[..., :, None] - b[..., None, :] + ij[..., None, :], -jnp.inf)
        inter_log = b + m[..., None]
        m_row = jnp.maximum(jnp.max(d_log, axis=-1), inter_log)
        w = jnp.exp(d_log - m_row[..., None])
        s = jnp.einsum('bhld,bhsd->bhls', qj, kj) * w
        inter_w = jnp.exp(inter_log - m_row)
        num = jnp.einsum('bhls,bhse->bhle', s, vj) + inter_w[..., None] * jnp.einsum('bhld,bhed->bhle', qj, c_mat)
        qn = jnp.sum(s, axis=-1) + inter_w * jnp.einsum('bhld,bhd->bhl', qj, n_vec)
        h = num / jnp.maximum(jnp.abs(qn), jnp.exp(-m_row))[..., None]
        b_last = b[..., -1]
        w_log = b_last[..., None] - b + ij
        m_new = jnp.maximum(b_last + m, jnp.max(w_log, axis=-1))
        ws = jnp.exp(w_log - m_new[..., None])
        carry_decay = jnp.exp(b_last + m - m_new)
        c_new = carry_decay[..., None, None] * c_mat + jnp.einsum('bhs,bhse,bhsd->bhed', ws, vj, kj)
        n_new = carry_decay[..., None] * n_vec + jnp.einsum('bhs,bhsd->bhd', ws, kj)
        return (c_new, n_new, m_new), h

    init = (jnp.zeros((bsz, nh, dh, dh), jnp.float32), jnp.zeros((bsz, nh, dh), jnp.float32),
            jnp.zeros((bsz, nh), jnp.float32))
    _, h = lax.scan(step, init, (qc, kc, vc, ic, fc))
    h = h.transpose(1, 0, 3, 2, 4).reshape(bsz, seq, nh, dh)
    h = jax.nn.sigmoid(o_raw).reshape(bsz, seq, nh, dh) * h
    h = _layernorm(h) * norm_w.reshape(nh, dh)
    return h.reshape(bsz, seq, GROUP_W)


def setup_inputs(seed: int = 0) -> dict:
    key = jax.random.key(seed)
    ks = jax.random.split(key, 20)
    f32 = jnp.float32
    nrm = jax.random.normal
    x = nrm(ks[0], (BATCH, SEQ, D_MODEL), f32)
    norm_w = 1.0 + 0.02 * nrm(ks[1], (DEPTH, D_MODEL), f32)
    w_in = nrm(ks[2], (DEPTH, D_MODEL, IN_COLS), f32) * (D_MODEL ** -0.5)
    w_out = nrm(ks[3], (DEPTH, D_MIX, D_MODEL), f32) * (D_MIX ** -0.5)
    diff_lambda = 0.1 * nrm(ks[4], (DEPTH, 4, DIFF_DK), f32)
    diff_norm_w = 1.0 + 0.02 * nrm(ks[5], (DEPTH, DIFF_DV), f32)
    ssd_conv_w = nrm(ks[6], (DEPTH, SSD_CONV, SSD_XBC), f32) * (SSD_CONV ** -0.5)
    ssd_conv_b = 0.02 * nrm(ks[7], (DEPTH, SSD_XBC), f32)
    u = jax.random.uniform(ks[8], (DEPTH, SSD_HEADS), f32)
    dt0 = jnp.exp(u * (math.log(0.1) - math.log(0.001)) + math.log(0.001))
    ssd_dt_bias = dt0 + jnp.log(-jnp.expm1(-dt0))
    ssd_a_log = jnp.log(jax.random.uniform(ks[9], (DEPTH, SSD_HEADS), f32, minval=1.0, maxval=16.0))
    ssd_d = 1.0 + 0.1 * nrm(ks[10], (DEPTH, SSD_HEADS), f32)
    ssd_norm_w = 1.0 + 0.02 * nrm(ks[11], (DEPTH, GROUP_W), f32)
    mlstm_conv_w = nrm(ks[12], (DEPTH, MLSTM_CONV, 2 * GROUP_W), f32) * (MLSTM_CONV ** -0.5)
    mlstm_conv_b = 0.02 * nrm(ks[13], (DEPTH, 2 * GROUP_W), f32)
    i_b = 0.1 * nrm(ks[14], (DEPTH, MLSTM_HEADS), f32)
    f_b = jnp.linspace(3.0, 6.0, MLSTM_HEADS, dtype=f32)[None, :] + 0.1 * nrm(ks[15], (DEPTH, MLSTM_HEADS), f32)
    mlstm_gate_b = jnp.concatenate([i_b, f_b], axis=-1)
    mlstm_norm_w = 1.0 + 0.02 * nrm(ks[16], (DEPTH, GROUP_W), f32)
    final_norm_w = 1.0 + 0.02 * nrm(ks[17], (D_MODEL,), f32)
    return {'x': x, 'norm_w': norm_w, 'w_in': w_in, 'w_out': w_out,
            'diff_lambda': diff_lambda, 'diff_norm_w': diff_norm_w,
            'ssd_conv_w': ssd_conv_w, 'ssd_conv_b': ssd_conv_b, 'ssd_dt_bias': ssd_dt_bias,
            'ssd_a_log': ssd_a_log, 'ssd_d': ssd_d, 'ssd_norm_w': ssd_norm_w,
            'mlstm_conv_w': mlstm_conv_w, 'mlstm_conv_b': mlstm_conv_b,
            'mlstm_gate_b': mlstm_gate_b, 'mlstm_norm_w': mlstm_norm_w,
            'final_norm_w': final_norm_w}


def reference(x, norm_w, w_in, w_out, diff_lambda, diff_norm_w, ssd_conv_w, ssd_conv_b, ssd_dt_bias,
              ssd_a_log, ssd_d, ssd_norm_w, mlstm_conv_w, mlstm_conv_b, mlstm_gate_b, mlstm_norm_w,
              final_norm_w):
    f32 = jnp.float32
    bsz, seq, _ = x.shape
    cos, sin = rope_tables(seq, RET_DK)
    silu = jax.nn.silu
    h = x
    for l in range(DEPTH):
        u = rmsnorm(h, norm_w[l]).astype(x.dtype)
        proj = jnp.einsum('bsd,de->bse', u, w_in[l]).astype(f32)
        ret_p, diff_p, ssd_p, ml_p = split_cols(proj, (RET_COLS, DIFF_COLS, SSD_COLS, MLSTM_COLS))

        rq, rk, rv, rz = split_cols(ret_p, (RET_HEADS * RET_DK, RET_HEADS * RET_DK, GROUP_W, GROUP_W))
        ret_out = retention_mixer(rq.reshape(bsz, seq, RET_HEADS, RET_DK),
                                  rk.reshape(bsz, seq, RET_HEADS, RET_DK),
                                  rv.reshape(bsz, seq, RET_HEADS, RET_DV), cos, sin) * silu(rz)

        dq, dk, dv, dz = split_cols(diff_p, (GROUP_W, GROUP_W, GROUP_W, GROUP_W))
        lam_init = 0.8 - 0.6 * math.exp(-0.3 * l)
        lp = diff_lambda[l].astype(f32)
        lam = jnp.exp(jnp.sum(lp[0] * lp[1])) - jnp.exp(jnp.sum(lp[2] * lp[3])) + lam_init
        diff_out = diff_attention_mixer(dq.reshape(bsz, seq, DIFF_HEADS, 2, DIFF_DK),
                                        dk.reshape(bsz, seq, DIFF_HEADS, 2, DIFF_DK),
                                        dv.reshape(bsz, seq, DIFF_HEADS, DIFF_DV),
                                        lam, lam_init, diff_norm_w[l].astype(f32), cos, sin) * silu(dz)

        xbc, dt_raw, sz = split_cols(ssd_p, (SSD_XBC, SSD_HEADS, GROUP_W))
        y = ssd_mixer(xbc, dt_raw, ssd_conv_w[l].astype(f32), ssd_conv_b[l].astype(f32),
                      ssd_dt_bias[l].astype(f32), ssd_a_log[l].astype(f32), ssd_d[l].astype(f32))
        ssd_out = _rms(y * silu(sz)) * ssd_norm_w[l].astype(f32)

        mqk, mv, mo, mg, mz = split_cols(ml_p, (2 * GROUP_W, GROUP_W, GROUP_W, 2 * MLSTM_HEADS, GROUP_W))
        ml_out = mlstm_mixer(mqk, mv, mo, mg, mlstm_conv_w[l].astype(f32), mlstm_conv_b[l].astype(f32),
                             mlstm_gate_b[l].astype(f32), mlstm_norm_w[l].astype(f32)) * silu(mz)

        mix = jnp.concatenate([ret_out, diff_out, ssd_out, ml_out], axis=-1).astype(x.dtype)
        h = h + jnp.einsum('bse,ed->bsd', mix, w_out[l]).astype(x.dtype)
    return rmsnorm(h, final_norm_w).astype(x.dtype)
```

```python
import math
from contextlib import ExitStack

import numpy as np
import concourse.bass as bass
import concourse.mybir as mybir
from concourse.bass_utils import run_bass_kernel_spmd

F32 = mybir.dt.float32
BF16 = mybir.dt.bfloat16
ALU = mybir.AluOpType
AF = mybir.ActivationFunctionType
AX = mybir.AxisListType

D_MODEL = 1024
CH = 128
EPS = 1e-6
RET_B, DIFF_B, SSD_B, ML_B = 0, 1536, 3584, 5128
NTM = 3084
NFM = 1536
NPAR = 924


class Buf:
    __slots__ = ("name", "last_w", "readers", "excl", "last_acc")

    def __init__(self, name="", excl=False):
        self.name = name
        self.last_w = None
        self.readers = []
        self.excl = excl
        self.last_acc = None


class Op:
    __slots__ = ("eng", "fn", "reads", "writes", "dma", "deps", "inc", "tick", "sem", "idx", "waits")

    def __init__(self, eng, fn, reads, writes, dma):
        self.eng = eng
        self.fn = fn
        self.reads = reads
        self.writes = writes
        self.dma = dma
        self.deps = []
        self.inc = False
        self.tick = None
        self.sem = None
        self.waits = []


class Sched:
    ENGS = ("pe", "act", "dve", "pool", "sp")
    DMA_RING = 8

    def __init__(self, nc):
        self.nc = nc
        self.ops = []

    def add(self, eng, fn, reads=(), writes=(), dma=False):
        op = Op(eng, fn, tuple(reads), tuple(writes), dma)
        op.idx = len(self.ops)
        self.ops.append(op)
        return op

    def pe(self, fn, r=(), w=()):
        return self.add("pe", fn, r, w)

    def act(self, fn, r=(), w=()):
        return self.add("act", fn, r, w)

    def dve(self, fn, r=(), w=()):
        return self.add("dve", fn, r, w)

    POOL_TO = "pool"

    def pool(self, fn, r=(), w=()):
        return self.add(self.POOL_TO, fn, r, w)

    def dma(self, eng, fn, r=(), w=()):
        return self.add(eng, fn, r, w, dma=True)

    def resolve(self, stack):
        ops = self.ops
        for op in ops:
            deps = {}
            for b in op.reads:
                if b.last_w is not None:
                    deps[b.last_w] = "raw"
            for b in op.writes:
                if b.last_w is not None and b.last_w not in deps:
                    deps[b.last_w] = "waw"
                for r in b.readers:
                    if r not in deps and r != op.idx:
                        deps[r] = "war"
            for b in op.reads + op.writes:
                if b.excl:
                    if b.last_acc is not None and ops[b.last_acc].eng != op.eng:
                        deps[b.last_acc] = "raw"
                    b.last_acc = op.idx
            for b in op.reads:
                b.readers.append(op.idx)
            for b in op.writes:
                b.last_w = op.idx
                b.readers = []
            for pi, kind in deps.items():
                p = ops[pi]
                if (not p.dma) and (not op.dma) and p.eng == op.eng:
                    if op.eng == "pe":
                        continue
                    if kind != "raw":
                        continue
                op.deps.append(pi)
                p.inc = True
        self.eng_sem = {}
        self.dma_sems = {}
        for e in self.ENGS:
            self.eng_sem[e] = stack.enter_context(self.nc.semaphore("tick_" + e))
        for e in ("sp", "pool", "act"):
            self.dma_sems[e] = [stack.enter_context(self.nc.semaphore("dma_%s_%d" % (e, i)))
                                for i in range(self.DMA_RING)]
        tick = {e: 0 for e in self.ENGS}
        dcount = {e: 0 for e in self.ENGS}
        for op in ops:
            if op.dma:
                n = dcount[op.eng]
                dcount[op.eng] += 1
                op.sem = self.dma_sems[op.eng][n % self.DMA_RING]
                op.tick = 16 * (n // self.DMA_RING + 1)
                op.inc = True
                if n >= self.DMA_RING:
                    op.waits.append((op.sem, op.tick - 16))
            elif op.inc:
                tick[op.eng] += 1
                op.sem = self.eng_sem[op.eng]
                op.tick = tick[op.eng]
        seen = {e: {} for e in self.ENGS}
        for op in ops:
            s = seen[op.eng]
            need = {}
            for (sem, val) in op.waits:
                need[id(sem)] = (sem, val)
            for pi in op.deps:
                p = ops[pi]
                k = id(p.sem)
                if k not in need or need[k][1] < p.tick:
                    need[k] = (p.sem, p.tick)
            op.waits = []
            for k, (sem, val) in need.items():
                if s.get(k, 0) >= val:
                    continue
                s[k] = val
                op.waits.append((sem, val))

    def emit(self, stack):
        self.resolve(stack)
        nc = self.nc
        per = {e: [op for op in self.ops if op.eng == e] for e in self.ENGS}
        block = stack.enter_context(nc.Block())

        def run(eng_handle, lst):
            for op in lst:
                for (sem, val) in op.waits:
                    eng_handle.wait_ge(sem, val)
                if op.fn is None:
                    continue
                ins = op.fn(eng_handle)
                if op.inc:
                    ins.then_inc(op.sem, 16 if op.dma else 1)

        @block.tensor
        def _(e):
            run(e, per["pe"])

        @block.scalar
        def _(e):
            run(e, per["act"])

        @block.vector
        def _(e):
            run(e, per["dve"])

        @block.gpsimd
        def _(e):
            run(e, per["pool"])

        @block.sync
        def _(e):
            run(e, per["sp"])


class T:
    def __init__(self, st, nc, name, shape, dt, psum=False):
        alloc = nc.psum_tensor if psum else nc.sbuf_tensor
        self.t = st.enter_context(alloc(name, shape, dt))
        self.b = Buf(name, excl=psum)

    def __getitem__(self, k):
        return self.t[k]


def _col_index(j):
    hs = (2 * j, 2 * j + 1)
    gs = (j, 1 - j)
    r = np.arange
    tm = []
    tm += [RET_B + h * 64 + r(64) for h in hs]
    tm += [RET_B + 256 + h * 64 + r(64) for h in hs]
    tm += [DIFF_B + h * 128 + r(128) for h in hs]
    tm += [DIFF_B + 512 + h * 128 + r(128) for h in hs]
    tm += [RET_B + 512 + h * 128 + r(128) for h in hs]
    tm += [DIFF_B + 1024 + h * 128 + r(128) for h in hs]
    tm += [ML_B + 1024 + h * 128 + r(128) for h in hs]
    tm += [RET_B + 1024 + h * 128 + r(128) for h in hs]
    tm += [DIFF_B + 1536 + h * 128 + r(128) for h in hs]
    tm += [SSD_B + 1032 + g * 256 + r(256) for g in gs]
    tm += [ML_B + 2056 + h * 128 + r(128) for h in hs]
    tm += [ML_B + 1536 + h * 128 + r(128) for h in hs]
    tm += [SSD_B + 1024 + g * 4 + r(4) for g in gs]
    tm += [ML_B + 2048 + 4 + np.array(hs)]
    tm += [ML_B + 2048 + np.array(hs)]
    tm = np.concatenate(tm)
    fm = []
    fm += [SSD_B + g * 256 + r(256) for g in gs]
    fm += [SSD_B + 512 + g * 128 + r(128) for g in gs]
    fm += [SSD_B + 768 + g * 128 + r(128) for g in gs]
    fm += [ML_B + h * 128 + r(128) for h in hs]
    fm += [ML_B + 512 + h * 128 + r(128) for h in hs]
    fm = np.concatenate(fm)
    assert tm.size == NTM and fm.size == NFM
    orow = np.concatenate([h * 128 + r(128) for h in hs] + [512 + h * 128 + r(128) for h in hs]
                          + [1024 + j * 256 + r(256)] + [1536 + h * 128 + r(128) for h in hs])
    return tm, fm, orow


def _consts(j):
    c = np.zeros((128, 776), np.float32)
    s = np.arange(128)[:, None]
    l = np.arange(128)[None, :]
    c[:, 0:128] = (l >= s)
    c[:, 128:256] = (s > l)
    for hh in range(2):
        h = 2 * j + hh
        lg = math.log(1.0 - 2.0 ** (-5.0 - h))
        c[:, 256 + hh * 128:256 + (hh + 1) * 128] = np.where(l >= s, np.exp(lg * np.maximum(l - s, 0)), 0.0) * 0.125
        c[:, 512 + hh] = np.exp(lg * (np.arange(128) + 1.0))
        c[:, 514 + hh] = np.exp(lg * (127.0 - np.arange(128))) * 0.125
        c[hh * 64:(hh + 1) * 64, 516] = np.exp(lg * 128.0)
    c[:, 517:645] = np.eye(128)
    c[:, 645:773] = 1.0
    return c


def _rope(S):
    inv = (10000.0 ** (-np.arange(0, 64, 2, dtype=np.float32) / np.float32(64))).astype(np.float32)
    ang = np.arange(S, dtype=np.float32)[:, None] * inv[None, :]
    cos, sin = np.cos(ang).astype(np.float32), np.sin(ang).astype(np.float32)
    return np.concatenate([cos, cos, -sin, sin], axis=1).astype(np.float32)


def build_layer(S, nprev, lam_init, debug=False, trunc=None, marks=None, noweights=False):
    NCH = S // CH
    nc = bass.Bass("TRN2", target_bir_lowering=False)
    dr = lambda n, shp, kind="ExternalInput": nc.dram_tensor(n, shp, F32, kind=kind).ap()
    x_d = dr("x", [S, D_MODEL])
    prev_d = [dr("prev%d" % i, [S, D_MODEL]) for i in range(nprev)]
    wtm_d = dr("wtm", [D_MODEL, NTM])
    wfm_d = dr("wfm", [D_MODEL, NFM])
    wout_d = dr("wout", [D_MODEL, D_MODEL])
    rope_d = dr("rope", [S, 128])
    par_d = dr("par", [1, NPAR])
    nwfm_d = dr("nwfm", [128, 8])
    convw_d = dr("convw", [128, 60])
    cst_d = dr("cst", [128, 776])
    out_d = dr("out", [S, D_MODEL], kind="ExternalOutput")
    dbg_d = dr("dbg", [S, D_MODEL], kind="ExternalOutput") if debug else None

    with ExitStack() as st:
        S_ = Sched(nc)
        sb = lambda n, shp, dt=F32: T(st, nc, n, shp, dt)
        wtm = sb("wtm_s", [128, 8, NTM], BF16)
        wfm = sb("wfm_s", [128, 8, NFM], BF16)
        wout = sb("wout_s", [128, 8, D_MODEL], BF16)
        kc = [sb("kc%d" % h, [128, S], BF16) for h in range(2)]
        vc = [sb("vc%d" % h, [128, NCH, 130], BF16) for h in range(2)]
        kcb = [[Buf() for _ in range(NCH)] for _ in range(2)]
        vcb = [[Buf() for _ in range(NCH)] for _ in range(2)]
        cst = sb("cst_s", [128, 776])
        cstb = sb("cstb_s", [128, 512], BF16)
        gbb = sb("gbb", [128, 2, 4, 128], BF16)
        hl = sb("hl", [128, 2, 16], BF16)
        par = sb("par_s", [128, 668])
        nwfm = sb("nwfm_s", [128, 8])
        convw = sb("convw_s", [128, 60])
        small = sb("small_s", [128, 64])
        rst = sb("rst", [128, 128])
        rstb = sb("rstb", [128, 128], BF16)
        sst = sb("sst", [128, 512])
        sstb = sb("sstb", [128, 512], BF16)
        mst = sb("mst", [128, 2, 130])
        mstb = sb("mstb", [128, 2, 130], BF16)
        rawfm = sb("rawfm", [128, 12, 131])
        mlv = sb("mlv", [128, 2, 130], BF16)
        hin = [sb("hin0", [128, D_MODEL])] * 2
        ropet = [sb("rope0", [128, 128])] * 2
        uT = sb("uT", [128, 8, 128], BF16)
        stat = sb("stat", [128, 32])
        f1 = sb("f1", [128, 512])
        f2 = sb("f2", [128, 512])
        f3 = sb("f3", [128, 512])
        qk0 = sb("qk0", [128, 512], BF16)
        kd = sb("kd", [128, 256], BF16)
        rqg = sb("rqg", [128, 128], BF16)
        rkg = sb("rkg", [128, 128], BF16)
        rv = sb("rv", [128, 256], BF16)
        rTk = sb("rTk", [128, 128], BF16)
        rTm = sb("rTm", [128, 4, 128], BF16)
        qdT = sb("qdT", [128, 2, 2, 128], BF16)
        zs = sb("zs", [128, 1280], BF16)
        sgo = sb("sgo", [128, 256], BF16)
        g6 = sb("g6", [128, 16])
        g6b = sb("g6b", [128, 16])
        dA = sb("dA", [128, 8])
        class _View:
            def __init__(self, base, view):
                self.b = base.b
                self.v = view

            def __getitem__(self, k):
                return self.v[k]
        fmc = _View(f3, f3[:].rearrange("p (a b) -> p a b", a=4))
        fmb = sb("fmb", [128, 12, 128], BF16)
        b1 = sb("b1", [128, 1024], BF16)
        b2 = sb("b2", [128, 1024], BF16)
        b3 = sb("b3", [128, 1024], BF16)
        pbf = [sb("pbf%d" % i, [128, 4, 128], BF16) for i in range(2)]
        mix = sb("mix", [128, D_MODEL], BF16)
        mixT = uT
        ubf = b3
        P = [T(st, nc, "P%d" % i, [128, 512], F32, psum=True) for i in range(7)]
        PT = T(st, nc, "PT", [128, 1024], BF16, psum=True)
        PTv = PT[:].rearrange("p (a b) -> p a b", a=8)

        maskT = cst[:, 0:128]
        Lst = cst[:, 128:256]
        onesf = cst[:, 645:773]
        ident = cstb[:, 128:256]
        maskTb = cstb[:, 0:128]
        Lstb = cstb[:, 256:384]
        onesb = cstb[:, 384:512]

        def v3(ap, a):
            return ap.rearrange("p (a b) -> p a b", a=a)

        def bc_last(ap2, n):
            return ap2.unsqueeze(2).broadcast_to([ap2.shape[0], ap2.shape[1], n])

        def bc_mid(ap2, a):
            return ap2.unsqueeze(1).broadcast_to([ap2.shape[0], a, ap2.shape[1]])

        S_.dma("sp", lambda e: e.dma_start(out=cst[:], in_=cst_d[:, :]), w=[cst.b])
        S_.dma("sp", lambda e: e.dma_start(out=par[:], in_=par_d[0:1, 0:668].broadcast_to([128, 668])), w=[par.b])
        S_.dma("sp", lambda e: e.dma_start(out=nwfm[:], in_=nwfm_d[:, :]), w=[nwfm.b])
        S_.dma("sp", lambda e: e.dma_start(out=convw[:], in_=convw_d[:, :]), w=[convw.b])
        stg = [f1, f2, f3]
        cnt = [0]

        def load_cast(dst, src_d, ncols):
            for k in range(0 if noweights else 8):
                for c0 in range(0, ncols, 512):
                    w_ = min(512, ncols - c0)
                    sg = stg[cnt[0] % 3]
                    ci = cnt[0] % 3
                    ce = (S_.dve, S_.act, S_.pool)[ci]
                    cnt[0] += 1
                    S_.dma("sp", lambda e, sg=sg, k=k, c0=c0, w_=w_: e.dma_start(out=sg[:, 0:w_], in_=src_d[k * 128:(k + 1) * 128, c0:c0 + w_]),
                           w=[sg.b])
                    if ci == 1:
                        ce(lambda e, sg=sg, k=k, c0=c0, w_=w_: e.copy(out=dst[:, k, c0:c0 + w_], in_=sg[:, 0:w_]), r=[sg.b], w=[dst.b])
                    else:
                        ce(lambda e, sg=sg, k=k, c0=c0, w_=w_: e.tensor_copy(out=dst[:, k, c0:c0 + w_], in_=sg[:, 0:w_]), r=[sg.b], w=[dst.b])

        load_cast(wtm, wtm_d, NTM)
        load_cast(wfm, wfm_d, NFM)
        load_cast(wout, wout_d, D_MODEL)
        S_.dve(lambda e: e.tensor_copy(out=cstb[:, 0:128], in_=cst[:, 0:128]), r=[cst.b], w=[cstb.b])
        S_.dve(lambda e: e.tensor_copy(out=cstb[:, 128:256], in_=cst[:, 517:645]), r=[cst.b], w=[cstb.b])
        S_.dve(lambda e: e.tensor_copy(out=cstb[:, 256:384], in_=cst[:, 128:256]), r=[cst.b], w=[cstb.b])
        S_.dve(lambda e: e.tensor_copy(out=cstb[:, 384:512], in_=cst[:, 645:773]), r=[cst.b], w=[cstb.b])
        for t_ in (rst, sst, mst, rawfm):
            S_.pool(lambda e, t_=t_: e.memset(t_[:], 0.0), w=[t_.b])
        for t_ in (rstb, sstb, mstb):
            S_.pool(lambda e, t_=t_: e.memset(t_[:], 0.0), w=[t_.b])
        S_.pool(lambda e: e.memset(mlv[:], 1.0), w=[mlv.b])
        S_.pool(lambda e: e.memset(rTm[:], 0.0), w=[rTm.b])
        S_.pool(lambda e: e.memset(qdT[:], 0.0), w=[qdT.b])
        for h in range(2):
            S_.pool(lambda e, h=h: e.memset(vc[h][:], 1.0), w=[vc[h].b] + vcb[h])
        PAR_DNW, PAR_SNW, PAR_MNW, PAR_B12, PAR_AL, PAR_DS, PAR_LAM = 0, 128, 384, 640, 652, 660, 668
        S_.act(lambda e: e.activation(out=small[:, 0:8], in_=par[:, PAR_AL:PAR_AL + 8], func=AF.Exp), r=[par.b], w=[small.b])
        S_.dve(lambda e: e.tensor_scalar(out=small[:, 0:8], in0=small[:, 0:8], scalar1=-1.0, scalar2=None, op0=ALU.mult),
               r=[small.b], w=[small.b])
        S_.dma("sp", lambda e: e.dma_start(out=f2[:, 0:256], in_=par_d[0:1, 668:924].broadcast_to([128, 256])), w=[f2.b])
        S_.dve(lambda e: e.tensor_tensor(out=f1[:, 0:128].rearrange("p (a b) -> p a b", a=2),
                                         in0=f2[:, 0:256].rearrange("p (a t b) -> p a t b", a=2, t=2)[:, :, 0, :],
                                         in1=f2[:, 0:256].rearrange("p (a t b) -> p a t b", a=2, t=2)[:, :, 1, :],
                                         op=ALU.mult), r=[f2.b], w=[f1.b])
        S_.dve(lambda e: e.tensor_reduce(out=small[:, 16:18], in_=f1[:, 0:128].rearrange("p (a b) -> p a b", a=2),
                                         axis=AX.X, op=ALU.add), r=[f1.b], w=[small.b])
        S_.act(lambda e: e.activation(out=small[:, 18:20], in_=small[:, 16:18], func=AF.Exp), r=[small.b], w=[small.b])
        S_.dve(lambda e: e.scalar_tensor_tensor(out=small[:, 20:21], in0=small[:, 19:20], scalar=-float(lam_init),
                                                in1=small[:, 18:19], op0=ALU.add, op1=ALU.subtract),
               r=[small.b], w=[small.b])
        S_.dve(lambda e: e.tensor_scalar(out=par[:, PAR_DNW:PAR_DNW + 128], in0=par[:, PAR_DNW:PAR_DNW + 128],
                                         scalar1=float(1.0 - lam_init), scalar2=None, op0=ALU.mult), r=[par.b], w=[par.b])

        R = {"P5a": P[5].b, "P5b": P[5].b, "P6a": P[6].b, "P6b": P[6].b}
        P5a, P5b, P6a, P6b = R["P5a"], R["P5b"], R["P6a"], R["P6b"]
        acc_i = [0]
        obs = []

        def rms_tail(src_ap3, nh, n, eps_in, dst_stat_col):
            c = dst_stat_col
            return c

        for t in range(NCH):
            hb = hin[t % 2]
            rp = ropet[t % 2]
            tok = slice(t * CH, (t + 1) * CH)
            if marks is not None:
                marks.append((t, '---------- A: load + rmsnorm', len(S_.ops)))
            S_.dma("sp", lambda e, hb=hb, tok=tok: e.dma_start(out=hb[:], in_=x_d[tok, :]), w=[hb.b])
            S_.dma("sp", lambda e, rp=rp, tok=tok: e.dma_start(out=rp[:], in_=rope_d[tok, :]), w=[rp.b])
            for i in range(nprev):
                for hf, sg in enumerate((f1, f2)):
                    S_.dma("sp", lambda e, i=i, tok=tok, hf=hf, sg=sg: e.dma_start(out=sg[:], in_=prev_d[i][tok, hf * 512:(hf + 1) * 512]),
                           w=[sg.b])
                    S_.pool(lambda e, hb=hb, hf=hf, sg=sg: e.tensor_tensor(out=hb[:, hf * 512:(hf + 1) * 512], in0=hb[:, hf * 512:(hf + 1) * 512],
                                                                           in1=sg[:], op=ALU.add), r=[hb.b, sg.b], w=[hb.b])
            S_.act(lambda e, hb=hb: e.activation(out=b2[:], in_=hb[:], func=AF.Square, scale=1.0 / 32.0,
                                                 accum_out=stat[:, 0:1]), r=[hb.b], w=[b2.b, stat.b])
            S_.dve(lambda e: e.tensor_scalar(out=stat[:, 0:1], in0=stat[:, 0:1], scalar1=EPS, scalar2=None, op0=ALU.add),
                   r=[stat.b], w=[stat.b])
            S_.act(lambda e: e.activation(out=stat[:, 1:2], in_=stat[:, 0:1], func=AF.Sqrt), r=[stat.b], w=[stat.b])
            S_.dve(lambda e: e.reciprocal(out=stat[:, 1:2], in_=stat[:, 1:2]), r=[stat.b], w=[stat.b])
            S_.dve(lambda e, hb=hb: e.tensor_scalar(out=ubf[:], in0=hb[:], scalar1=stat[:, 1:2], scalar2=None, op0=ALU.mult),
                   r=[hb.b, stat.b], w=[ubf.b])
            for k in range(8):
                S_.pe(lambda e, k=k: e.transpose(out=PTv[:, k, :], in_=ubf[:, k * 128:(k + 1) * 128], identity=ident),
                      r=[ubf.b, cstb.b], w=[PT.b])
            S_.dve(lambda e: e.tensor_tensor(out=uT[:], in0=PTv, in1=bc_last(nwfm[:, 0:8], 128), op=ALU.mult),
                   r=[PT.b, nwfm.b], w=[uT.b])

            if marks is not None:
                marks.append((t, '---------- B: in-proj ------', len(S_.ops)))
            def tm_group(g, width, bank):
                for k in range(8):
                    S_.pe(lambda e, k=k: e.matmul(bank[:, 0:width], lhsT=uT[:, k, :], rhs=wtm[:, k, g * 512:g * 512 + width],
                                                  start=(k == 0), stop=(k == 7)), r=[uT.b, wtm.b], w=[bank.b])

            def rope(bank, ncols, dst_ap, extra_w):
                nh = ncols // 64
                src4 = bank[:, 0:ncols].rearrange("p (h t d) -> p h t d", h=nh, t=2)
                S_.dve(lambda e: e.tensor_tensor(out=v3(f1[:, 0:ncols], nh), in0=v3(bank[:, 0:ncols], nh),
                                                 in1=bc_mid(rp[:, 0:64], nh), op=ALU.mult), r=[bank.b, rp.b], w=[f1.b])
                f24 = f2[:, 0:ncols].rearrange("p (h t d) -> p h t d", h=nh, t=2)
                S_.dve(lambda e: e.tensor_tensor(out=f24[:, :, 0, :], in0=src4[:, :, 1, :],
                                                 in1=bc_mid(rp[:, 64:96], nh), op=ALU.mult), r=[bank.b, rp.b], w=[f2.b])
                S_.dve(lambda e: e.tensor_tensor(out=f24[:, :, 1, :], in0=src4[:, :, 0, :],
                                                 in1=bc_mid(rp[:, 96:128], nh), op=ALU.mult), r=[bank.b, rp.b], w=[f2.b])
                S_.pool(lambda e: e.tensor_tensor(out=dst_ap, in0=f1[:, 0:ncols], in1=f2[:, 0:ncols], op=ALU.add),
                        r=[f1.b, f2.b], w=extra_w)

            tm_group(0, 512, P[0])
            rope(P[0], 512, qk0[:], [qk0.b])
            tm_group(1, 512, P[1])
            rope(P[1], 256, kd[:], [kd.b])
            S_.act(lambda e: e.copy(out=rv[:], in_=P[1][:, 256:512]), r=[P[1].b], w=[rv.b])
            tm_group(2, 512, P[0])
            for h in range(2):
                S_.act(lambda e, h=h, t=t: e.copy(out=vc[h][:, t, 0:128], in_=P[0][:, h * 128:(h + 1) * 128]),
                       r=[P[0].b], w=[vcb[h][t]])
            S_.act(lambda e: e.copy(out=mlv[:, :, 0:128], in_=v3(P[0][:, 256:512], 2)), r=[P[0].b], w=[mlv.b])
            tm_group(3, 512, P[1])
            S_.act(lambda e: e.activation(out=zs[:, 0:512], in_=P[1][:, 0:512], func=AF.Silu), r=[P[1].b], w=[zs.b])
            tm_group(4, 512, P[0])
            S_.act(lambda e: e.activation(out=zs[:, 512:1024], in_=P[0][:, 0:512], func=AF.Silu), r=[P[0].b], w=[zs.b])
            tm_group(5, 512, P[1])
            S_.act(lambda e: e.activation(out=zs[:, 1024:1280], in_=P[1][:, 0:256], func=AF.Silu), r=[P[1].b], w=[zs.b])
            S_.act(lambda e: e.activation(out=sgo[:], in_=P[1][:, 256:512], func=AF.Sigmoid), r=[P[1].b], w=[sgo.b])
            tm_group(6, 12, P[0])
            S_.dve(lambda e: e.tensor_tensor(out=g6[:, 0:12], in0=P[0][:, 0:12], in1=par[:, PAR_B12:PAR_B12 + 12], op=ALU.add),
                   r=[P[0].b, par.b], w=[g6.b])
            S_.act(lambda e: e.activation(out=g6b[:, 0:8], in_=g6[:, 0:8], func=AF.Exp), r=[g6.b], w=[g6b.b])
            S_.act(lambda e: e.activation(out=g6b[:, 8:10], in_=g6[:, 8:10], func=AF.Exp, scale=-1.0), r=[g6.b], w=[g6b.b])
            S_.act(lambda e: e.activation(out=g6b[:, 0:10], in_=g6b[:, 0:10], func=AF.Ln, bias=1.0), r=[g6b.b], w=[g6b.b])
            S_.dve(lambda e: e.tensor_tensor(out=dA[:], in0=g6b[:, 0:8], in1=small[:, 0:8], op=ALU.mult),
                   r=[g6b.b, small.b], w=[dA.b])
            S_.dve(lambda e: e.tensor_scalar(out=g6b[:, 8:10], in0=g6b[:, 8:10], scalar1=-1.0, scalar2=None, op0=ALU.mult),
                   r=[g6b.b], w=[g6b.b])
            if marks is not None:
                marks.append((t, 'FM blocks (+ depthwise', len(S_.ops)))
            for q4 in range(3):
                bank = P[1] if q4 % 2 == 0 else P[0]
                for bb in range(4):
                    blk = q4 * 4 + bb
                    for k in range(8):
                        S_.pe(lambda e, k=k, blk=blk, bb=bb, bank=bank: e.matmul(
                            bank[:, bb * 128:(bb + 1) * 128], lhsT=wfm[:, k, blk * 128:(blk + 1) * 128], rhs=uT[:, k, :],
                            start=(k == 0), stop=(k == 7)), r=[uT.b, wfm.b], w=[bank.b])
                S_.act(lambda e, q4=q4, bank=bank: e.copy(out=rawfm[:, q4 * 4:(q4 + 1) * 4, 3:131], in_=v3(bank[:, 0:512], 4)),
                       r=[bank.b], w=[rawfm.b])
                for bb in range(4):
                    blk = q4 * 4 + bb
                    eng = S_.dve
                    cw = convw[:, blk * 5:(blk + 1) * 5]
                    eng(lambda e, blk=blk, bb=bb, cw=cw: e.tensor_scalar(out=fmc[:, bb, :], in0=rawfm[:, blk, 0:128], scalar1=cw[:, 0:1],
                                                                        scalar2=cw[:, 4:5], op0=ALU.mult, op1=ALU.add),
                        r=[rawfm.b, convw.b], w=[fmc.b])
                    for k in range(1, 4):
                        eng(lambda e, blk=blk, bb=bb, cw=cw, k=k: e.scalar_tensor_tensor(out=fmc[:, bb, :], in0=rawfm[:, blk, k:k + 128],
                                                                                      scalar=cw[:, k:k + 1], in1=fmc[:, bb, :],
                                                                                      op0=ALU.mult, op1=ALU.add),
                            r=[rawfm.b, convw.b, fmc.b], w=[fmc.b])
                S_.act(lambda e, q4=q4: e.activation(out=fmb[:, q4 * 4:(q4 + 1) * 4, :], in_=fmc[:], func=AF.Silu), r=[fmc.b], w=[fmb.b])
            S_.pool(lambda e: e.tensor_copy(out=rawfm[:, :, 0:3], in_=rawfm[:, :, 128:131]), r=[rawfm.b], w=[rawfm.b])

            if marks is not None:
                marks.append((t, '---------- C: retention ----', len(S_.ops)))
            S_.pool(lambda e: e.tensor_tensor(out=v3(rqg[:], 2), in0=v3(qk0[:, 0:128], 2), in1=bc_last(cst[:, 512:514], 64),
                                              op=ALU.mult), r=[qk0.b, cst.b], w=[rqg.b])
            S_.pool(lambda e: e.tensor_tensor(out=v3(rkg[:], 2), in0=v3(qk0[:, 128:256], 2), in1=bc_last(cst[:, 514:516], 64),
                                              op=ALU.mult), r=[qk0.b, cst.b], w=[rkg.b])
            S_.pe(lambda e: e.transpose(out=PTv[:, 0, :], in_=qk0[:, 0:128], identity=ident), r=[qk0.b, cstb.b], w=[PT.b])
            S_.pe(lambda e: e.transpose(out=PTv[:, 1, :], in_=qk0[:, 128:256], identity=ident), r=[qk0.b, cstb.b], w=[PT.b])
            S_.pe(lambda e: e.transpose(out=PTv[:, 2, :], in_=rqg[:], identity=ident), r=[rqg.b, cstb.b], w=[PT.b])
            S_.act(lambda e: e.copy(out=rTk[:], in_=PTv[:, 1, :]), r=[PT.b], w=[rTk.b])
            S_.act(lambda e: e.copy(out=rTm[0:64, 0:4:2, :], in_=PTv[0:64, 0:3:2, :]), r=[PT.b], w=[rTm.b])
            S_.act(lambda e: e.copy(out=rTm[64:128, 1:4:2, :], in_=PTv[64:128, 0:3:2, :]), r=[PT.b], w=[rTm.b])
            for h in range(2):
                S_.pe(lambda e, h=h: e.matmul(P[5][:, h * 128:(h + 1) * 128], lhsT=rTk[:], rhs=rTm[:, h, :],
                                              start=True, stop=True), r=[rTk.b, rTm.b], w=[P5a])
            S_.dve(lambda e: e.tensor_tensor(out=b1[:, 0:256], in0=P[5][:, 0:256], in1=cst[:, 256:512], op=ALU.mult),
                   r=[P5a, cst.b], w=[b1.b])
            for h in range(2):
                S_.pe(lambda e, h=h: e.matmul(P[5][:, 256 + h * 128:256 + (h + 1) * 128], lhsT=b1[:, h * 128:(h + 1) * 128],
                                              rhs=rv[:, h * 128:(h + 1) * 128], start=True, stop=False), r=[b1.b, rv.b], w=[P5b])
                S_.pe(lambda e, h=h: e.matmul(P[5][:, 256 + h * 128:256 + (h + 1) * 128], lhsT=rTm[:, 2 + h, :],
                                              rhs=rstb[:], start=False, stop=True), r=[rTm.b, rstb.b], w=[P5b])
            for h in range(2):
                S_.pe(lambda e, h=h: e.matmul(P[6][:, h * 128:(h + 1) * 128], lhsT=rkg[:],
                                              rhs=rv[:, h * 128:(h + 1) * 128], start=True, stop=True),
                      r=[rkg.b, rv.b], w=[P6a])
            for h in range(2):
                ps = slice(h * 64, (h + 1) * 64)
                S_.dve(lambda e, h=h, ps=ps: e.scalar_tensor_tensor(out=rst[ps, :], in0=rst[ps, :], scalar=cst[ps, 516:517],
                                                                    in1=P[6][ps, h * 128:(h + 1) * 128],
                                                                    op0=ALU.mult, op1=ALU.add), r=[rst.b, cst.b, P6a], w=[rst.b])
            S_.act(lambda e: e.copy(out=rstb[:], in_=rst[:]), r=[rst.b], w=[rstb.b])
            S_.act(lambda e: e.activation(out=f1[:, 0:256], in_=P[5][:, 256:512], func=AF.Square, scale=1.0 / math.sqrt(128.0)),
                   r=[P5b], w=[f1.b])
            S_.dve(lambda e: e.tensor_reduce(out=stat[:, 2:4], in_=v3(f1[:, 0:256], 2), axis=AX.X, op=ALU.add),
                   r=[f1.b], w=[stat.b])
            S_.dve(lambda e: e.tensor_scalar(out=stat[:, 2:4], in0=stat[:, 2:4], scalar1=EPS, scalar2=None, op0=ALU.add),
                   r=[stat.b], w=[stat.b])
            S_.act(lambda e: e.activation(out=stat[:, 2:4], in_=stat[:, 2:4], func=AF.Sqrt), r=[stat.b], w=[stat.b])
            S_.dve(lambda e: e.reciprocal(out=stat[:, 2:4], in_=stat[:, 2:4]), r=[stat.b], w=[stat.b])
            S_.dve(lambda e: e.tensor_tensor(out=v3(f2[:, 0:256], 2), in0=v3(P[5][:, 256:512], 2), in1=bc_last(stat[:, 2:4], 128),
                                             op=ALU.mult), r=[P5b, stat.b], w=[f2.b])
            S_.pool(lambda e: e.tensor_tensor(out=mix[:, 0:256], in0=f2[:, 0:256], in1=zs[:, 0:256], op=ALU.mult),
                    r=[f2.b, zs.b], w=[mix.b])

            if marks is not None:
                marks.append((t, '---------- E: SSD ----------', len(S_.ops)))
            for i in range(6):
                S_.pe(lambda e, i=i: e.transpose(out=PTv[:, i, :], in_=fmb[:, i, :], identity=ident), r=[fmb.b, cstb.b], w=[PT.b])
            S_.act(lambda e: e.copy(out=b2[:, 0:768], in_=PT[:, 0:768]), r=[PT.b], w=[b2.b])
            S_.pool(lambda e: e.tensor_tensor(out=v3(b3[:, 0:512], 8), in0=v3(b2[:, 0:512], 8), in1=bc_last(g6b[:, 0:8], 64),
                                              op=ALU.mult), r=[b2.b, g6b.b], w=[b3.b])
            S_.dve(lambda e: e.tensor_copy(out=hl[:, 0, 0:8], in_=dA[:, 0:8]), r=[dA.b], w=[hl.b])
            S_.dve(lambda e: e.tensor_tensor(out=hl[:, 1, 0:8], in0=dA[:, 0:8], in1=hl[:, 0, 0:8], op=ALU.subtract), r=[dA.b, hl.b], w=[hl.b])
            for g in range(2):
                S_.pool(lambda e, g=g: e.tensor_tensor(
                    out=gbb[:], in0=maskTb.unsqueeze(1).unsqueeze(1).broadcast_to([128, 2, 4, 128]),
                    in1=hl[:, :, g * 4:(g + 1) * 4].unsqueeze(3).broadcast_to([128, 2, 4, 128]), op=ALU.mult),
                    r=[cstb.b, hl.b], w=[gbb.b])
                for q in range(2):
                    S_.pe(lambda e, g=g, q=q: e.matmul(P[5 + g][:, 0:512], lhsT=Lstb, rhs=gbb[:, q, :, :].rearrange("p a b -> p (a b)"),
                                                       start=(q == 0), stop=(q == 1)), r=[cstb.b, gbb.b], w=[R["P%da" % (5 + g)], R["P%db" % (5 + g)]])
            for g in range(2):
                S_.act(lambda e, g=g: e.activation(out=b1[:, g * 512:(g + 1) * 512], in_=P[5 + g][:, 0:512], func=AF.Exp),
                       r=[R["P%da" % (5 + g)], R["P%db" % (5 + g)]], w=[b1.b])
                S_.act(lambda e, g=g: e.activation(out=stat[:, 8 + g * 4:12 + g * 4], in_=v3(P[5 + g][:, 0:512], 4)[:, :, 127],
                                                   func=AF.Exp), r=[R["P%da" % (5 + g)], R["P%db" % (5 + g)]], w=[stat.b])
            for q in range(2):
                S_.pe(lambda e, q=q: e.matmul(P[5][:, 0:8], lhsT=maskTb, rhs=hl[:, q, 0:8], start=(q == 0), stop=(q == 1)), r=[cstb.b, hl.b], w=[P5a, P5b])
            for q in range(2):
                S_.pe(lambda e, q=q: e.matmul(P[5][:, 8:16], lhsT=onesb, rhs=hl[:, q, 0:8], start=(q == 0), stop=(q == 1)), r=[cstb.b, hl.b], w=[P5a, P5b])
            S_.act(lambda e: e.activation(out=stat[:, 16:32], in_=P[5][:, 0:16], func=AF.Exp), r=[P5a, P5b], w=[stat.b])
            for g in range(2):
                S_.pe(lambda e, g=g: e.matmul(P[6][:, g * 128:(g + 1) * 128], lhsT=fmb[:, 4 + g, :], rhs=fmb[:, 6 + g, :],
                                              start=True, stop=True), r=[fmb.b], w=[P6a, P6b])
            S_.dve(lambda e: e.tensor_tensor(out=v3(f1[:, 0:256], 2), in0=v3(P[6][:, 0:256], 2), in1=bc_mid(maskT, 2), op=ALU.mult),
                   r=[P6a, P6b, cst.b], w=[f1.b])
            for g in range(2):
                S_.pool(lambda e, g=g: e.tensor_tensor(out=v3(b1[:, g * 512:(g + 1) * 512], 4), in0=v3(b1[:, g * 512:(g + 1) * 512], 4),
                                                       in1=bc_mid(f1[:, g * 128:(g + 1) * 128], 4), op=ALU.mult),
                        r=[b1.b, f1.b], w=[b1.b])
            S_.pool(lambda e: e.tensor_tensor(out=v3(b3[:, 512:1024], 8), in0=v3(b3[:, 0:512], 8), in1=bc_last(stat[:, 8:16], 64),
                                              op=ALU.mult), r=[b3.b, stat.b], w=[b3.b])
            for hh in range(8):
                S_.pe(lambda e, hh=hh: e.matmul(P[5][:, hh * 64:(hh + 1) * 64], lhsT=b1[:, hh * 128:(hh + 1) * 128],
                                                rhs=b3[:, hh * 64:(hh + 1) * 64], start=True, stop=True),
                      r=[b1.b, b3.b], w=[P5a, P5b])
            for g in range(2):
                S_.pe(lambda e, g=g: e.matmul(P[6][:, g * 256:(g + 1) * 256], lhsT=fmb[:, 6 + g, :], rhs=sstb[:, g * 256:(g + 1) * 256],
                                              start=True, stop=True), r=[fmb.b, sstb.b], w=[P6a, P6b])
            S_.dve(lambda e: e.tensor_tensor(out=v3(f2[:], 8), in0=v3(P[6][:, 0:512], 8), in1=bc_last(stat[:, 16:24], 64), op=ALU.mult),
                   r=[P6a, P6b, stat.b], w=[f2.b])
            S_.dve(lambda e: e.tensor_tensor(out=f2[:], in0=P[5][:, 0:512], in1=f2[:], op=ALU.add), r=[P5a, P5b, f2.b], w=[f2.b])
            S_.pool(lambda e: e.tensor_tensor(out=v3(f3[:], 8), in0=v3(b2[:, 0:512], 8), in1=bc_last(par[:, PAR_DS:PAR_DS + 8], 64),
                                              op=ALU.mult), r=[b2.b, par.b], w=[f3.b])
            S_.pool(lambda e: e.tensor_tensor(out=f2[:], in0=f2[:], in1=f3[:], op=ALU.add), r=[f2.b, f3.b], w=[f2.b])
            S_.pool(lambda e: e.tensor_tensor(out=f2[:], in0=f2[:], in1=zs[:, 512:1024], op=ALU.mult), r=[f2.b, zs.b], w=[f2.b])
            for g in range(2):
                S_.pe(lambda e, g=g: e.matmul(P[6][:, g * 256:(g + 1) * 256], lhsT=b2[:, 512 + g * 128:512 + (g + 1) * 128],
                                              rhs=b3[:, 512 + g * 256:512 + (g + 1) * 256], start=True, stop=True),
                      r=[b2.b, b3.b], w=[P6a, P6b])
            S_.pool(lambda e: e.tensor_tensor(out=v3(sst[:], 8), in0=v3(sst[:], 8), in1=bc_last(stat[:, 24:32], 64), op=ALU.mult),
                    r=[sst.b, stat.b], w=[sst.b])
            S_.dve(lambda e: e.tensor_tensor(out=sst[:], in0=sst[:], in1=P[6][:, 0:512], op=ALU.add), r=[sst.b, P6a, P6b], w=[sst.b])
            S_.act(lambda e: e.copy(out=sstb[:], in_=sst[:]), r=[sst.b], w=[sstb.b])
            S_.act(lambda e: e.activation(out=f3[:], in_=f2[:], func=AF.Square, scale=1.0 / math.sqrt(512.0), accum_out=stat[:, 4:5]),
                   r=[f2.b], w=[f3.b, stat.b])
            S_.dve(lambda e: e.tensor_scalar(out=stat[:, 4:5], in0=stat[:, 4:5], scalar1=EPS, scalar2=None, op0=ALU.add),
                   r=[stat.b], w=[stat.b])
            S_.act(lambda e: e.activation(out=stat[:, 4:5], in_=stat[:, 4:5], func=AF.Sqrt), r=[stat.b], w=[stat.b])
            S_.dve(lambda e: e.reciprocal(out=stat[:, 4:5], in_=stat[:, 4:5]), r=[stat.b], w=[stat.b])
            S_.dve(lambda e: e.scalar_tensor_tensor(out=mix[:, 512:768], in0=f2[:, 0:256], scalar=stat[:, 4:5],
                                                    in1=par[:, PAR_SNW:PAR_SNW + 256], op0=ALU.mult, op1=ALU.mult),
                   r=[f2.b, stat.b, par.b], w=[mix.b])

            if marks is not None:
                marks.append((t, '---------- F: mLSTM --------', len(S_.ops)))
            for h in range(2):
                S_.pe(lambda e, h=h: e.transpose(out=PTv[:, h, :], in_=fmb[:, 10 + h, :], identity=ident), r=[fmb.b, cstb.b], w=[PT.b])
            S_.act(lambda e: e.copy(out=b2[:, 768:1024], in_=PT[:, 0:256]), r=[PT.b], w=[b2.b])
            S_.dve(lambda e: e.tensor_copy(out=hl[:, 0, 8:10], in_=g6b[:, 8:10]), r=[g6b.b], w=[hl.b])
            S_.dve(lambda e: e.tensor_tensor(out=hl[:, 1, 8:10], in0=g6b[:, 8:10], in1=hl[:, 0, 8:10], op=ALU.subtract), r=[g6b.b, hl.b], w=[hl.b])
            S_.pool(lambda e: e.tensor_tensor(
                out=gbb[:, :, 0:2, :], in0=maskTb.unsqueeze(1).unsqueeze(1).broadcast_to([128, 2, 2, 128]),
                in1=hl[:, :, 8:10].unsqueeze(3).broadcast_to([128, 2, 2, 128]), op=ALU.mult), r=[cstb.b, hl.b], w=[gbb.b])
            for q in range(2):
                S_.pe(lambda e, q=q: e.matmul(P[5][:, 0:256], lhsT=Lstb, rhs=gbb[:, q, 0:2, :].rearrange("p a b -> p (a b)"),
                                              start=(q == 0), stop=(q == 1)), r=[cstb.b, gbb.b], w=[P5a])
            for h in range(2):
                S_.act(lambda e, h=h: e.activation(out=f1[:, h * 128:(h + 1) * 128], in_=P[5][:, h * 128:(h + 1) * 128], func=AF.Exp,
                                                   bias=g6[:, 10 + h:11 + h]), r=[P5a, g6.b], w=[f1.b])
            S_.dve(lambda e: e.scalar_tensor_tensor(out=v3(f1[:, 0:256], 2), in0=v3(f1[:, 0:256], 2), scalar=1.0 / math.sqrt(128.0),
                                                     in1=bc_mid(maskT, 2), op0=ALU.mult, op1=ALU.mult), r=[f1.b, cst.b], w=[f1.b])
            for h in range(2):
                S_.pe(lambda e, h=h: e.matmul(P[5][:, 256 + h * 128:256 + (h + 1) * 128], lhsT=fmb[:, 10 + h, :], rhs=fmb[:, 8 + h, :],
                                              start=True, stop=True), r=[fmb.b], w=[P5b])
            S_.dve(lambda e: e.tensor_tensor(out=b1[:, 0:256], in0=P[5][:, 256:512], in1=f1[:, 0:256], op=ALU.mult),
                   r=[P5b, f1.b], w=[b1.b])
            for q in range(2):
                S_.pe(lambda e, q=q: e.matmul(P[6][:, 0:2], lhsT=maskTb, rhs=hl[:, q, 8:10], start=(q == 0), stop=(q == 1)), r=[cstb.b, hl.b], w=[P6a])
            for q in range(2):
                S_.pe(lambda e, q=q: e.matmul(P[6][:, 2:4], lhsT=onesb, rhs=hl[:, q, 8:10], start=(q == 0), stop=(q == 1)), r=[cstb.b, hl.b], w=[P6a])
            S_.act(lambda e: e.activation(out=stat[:, 8:12], in_=P[6][:, 0:4], func=AF.Exp), r=[P6a], w=[stat.b])
            S_.dve(lambda e: e.tensor_tensor(out=stat[:, 12:14], in0=g6[:, 10:12], in1=P[6][:, 0:2], op=ALU.subtract),
                   r=[g6.b, P6a], w=[stat.b])
            S_.dve(lambda e: e.tensor_tensor(out=stat[:, 12:14], in0=stat[:, 12:14], in1=P[6][:, 2:4], op=ALU.add),
                   r=[stat.b, P6a], w=[stat.b])
            S_.act(lambda e: e.activation(out=stat[:, 12:14], in_=stat[:, 12:14], func=AF.Exp), r=[stat.b], w=[stat.b])
            P5bv = P[5][:, 0:260].rearrange("p (a b) -> p a b", a=2)
            P6bv = P[6][:, 0:260].rearrange("p (a b) -> p a b", a=2)
            for h in range(2):
                S_.pe(lambda e, h=h: e.matmul(P5bv[:, h, :], lhsT=b1[:, h * 128:(h + 1) * 128], rhs=mlv[:, h, :], start=True, stop=True),
                      r=[b1.b, mlv.b], w=[P5a, P5b])
            for h in range(2):
                S_.pe(lambda e, h=h: e.matmul(P6bv[:, h, :], lhsT=fmb[:, 8 + h, :], rhs=mstb[:, h, :], start=True, stop=True),
                      r=[fmb.b, mstb.b], w=[P6a, P6b])
            f1v = f1[:, 0:260].rearrange("p (a b) -> p a b", a=2)
            S_.act(lambda e: e.copy(out=f1v, in_=P5bv), r=[P5a, P5b], w=[f1.b])
            for h in range(2):
                S_.dve(lambda e, h=h: e.scalar_tensor_tensor(out=f1v[:, h, :], in0=P6bv[:, h, :], scalar=stat[:, 8 + h:9 + h],
                                                             in1=f1v[:, h, :], op0=ALU.mult, op1=ALU.add),
                       r=[P6a, P6b, stat.b, f1.b], w=[f1.b])
            S_.dve(lambda e: e.scalar_tensor_tensor(out=v3(b3[:, 0:260], 2), in0=mlv[:], scalar=1.0 / math.sqrt(128.0),
                                                     in1=bc_last(stat[:, 12:14], 130), op0=ALU.mult, op1=ALU.mult),
                    r=[mlv.b, stat.b], w=[b3.b])
            for h in range(2):
                S_.pe(lambda e, h=h: e.matmul(P6bv[:, h, :], lhsT=b2[:, 768 + h * 128:768 + (h + 1) * 128],
                                              rhs=b3[:, h * 130:(h + 1) * 130], start=True, stop=True), r=[b2.b, b3.b], w=[P6a, P6b])
            S_.pool(lambda e: e.tensor_tensor(out=mst[:], in0=mst[:], in1=bc_last(stat[:, 10:12], 130), op=ALU.mult),
                    r=[mst.b, stat.b], w=[mst.b])
            S_.dve(lambda e: e.tensor_tensor(out=mst[:], in0=mst[:], in1=P6bv, op=ALU.add), r=[mst.b, P6a, P6b], w=[mst.b])
            S_.act(lambda e: e.copy(out=mstb[:], in_=mst[:]), r=[mst.b], w=[mstb.b])
            S_.act(lambda e: e.activation(out=stat[:, 14:16], in_=f1v[:, :, 128], func=AF.Abs), r=[f1.b], w=[stat.b])
            S_.dve(lambda e: e.tensor_scalar(out=stat[:, 14:16], in0=stat[:, 14:16], scalar1=1.0, scalar2=None, op0=ALU.max),
                   r=[stat.b], w=[stat.b])
            S_.dve(lambda e: e.reciprocal(out=stat[:, 14:16], in_=stat[:, 14:16]), r=[stat.b], w=[stat.b])
            S_.dve(lambda e: e.tensor_tensor(out=v3(f2[:, 0:256], 2), in0=f1v[:, :, 0:128], in1=bc_last(stat[:, 14:16], 128), op=ALU.mult),
                   r=[f1.b, stat.b], w=[f2.b])
            S_.pool(lambda e: e.tensor_tensor(out=f2[:, 0:256], in0=f2[:, 0:256], in1=sgo[:], op=ALU.mult), r=[f2.b, sgo.b], w=[f2.b])
            S_.dve(lambda e: e.tensor_reduce(out=stat[:, 5:7], in_=v3(f2[:, 0:256], 2), axis=AX.X, op=ALU.add), r=[f2.b], w=[stat.b])
            S_.dve(lambda e: e.tensor_scalar(out=stat[:, 5:7], in0=stat[:, 5:7], scalar1=-1.0 / 128.0, scalar2=None, op0=ALU.mult),
                   r=[stat.b], w=[stat.b])
            S_.dve(lambda e: e.tensor_tensor(out=v3(f2[:, 0:256], 2), in0=v3(f2[:, 0:256], 2), in1=bc_last(stat[:, 5:7], 128), op=ALU.add),
                   r=[f2.b, stat.b], w=[f2.b])
            S_.act(lambda e: e.activation(out=f3[:, 0:256], in_=f2[:, 0:256], func=AF.Square, scale=1.0 / math.sqrt(128.0)),
                   r=[f2.b], w=[f3.b])
            S_.dve(lambda e: e.tensor_reduce(out=stat[:, 5:7], in_=v3(f3[:, 0:256], 2), axis=AX.X, op=ALU.add), r=[f3.b], w=[stat.b])
            S_.dve(lambda e: e.tensor_scalar(out=stat[:, 5:7], in0=stat[:, 5:7], scalar1=EPS, scalar2=None, op0=ALU.add),
                   r=[stat.b], w=[stat.b])
            S_.act(lambda e: e.activation(out=stat[:, 5:7], in_=stat[:, 5:7], func=AF.Sqrt), r=[stat.b], w=[stat.b])
            S_.dve(lambda e: e.reciprocal(out=stat[:, 5:7], in_=stat[:, 5:7]), r=[stat.b], w=[stat.b])
            S_.dve(lambda e: e.tensor_tensor(out=v3(f2[:, 0:256], 2), in0=v3(f2[:, 0:256], 2), in1=bc_last(stat[:, 5:7], 128), op=ALU.mult),
                   r=[f2.b, stat.b], w=[f2.b])
            S_.pool(lambda e: e.tensor_tensor(out=f2[:, 0:256], in0=f2[:, 0:256], in1=par[:, PAR_MNW:PAR_MNW + 256], op=ALU.mult),
                    r=[f2.b, par.b], w=[f2.b])
            S_.pool(lambda e: e.tensor_tensor(out=mix[:, 768:1024], in0=f2[:, 0:256], in1=zs[:, 1024:1280], op=ALU.mult),
                    r=[f2.b, zs.b], w=[mix.b])

            if marks is not None:
                marks.append((t, '---------- D: differential a', len(S_.ops)))
            for h in range(2):
                S_.pe(lambda e, h=h: e.transpose(out=PTv[:, h, :], in_=qk0[:, 256 + h * 128:256 + (h + 1) * 128], identity=ident),
                      r=[qk0.b, cstb.b], w=[PT.b])
                S_.pe(lambda e, h=h: e.transpose(out=PTv[:, 2 + h, :], in_=kd[:, h * 128:(h + 1) * 128], identity=ident),
                      r=[kd.b, cstb.b], w=[PT.b])
            S_.act(lambda e: e.copy(out=qdT[0:64, :, 0, :], in_=PTv[0:64, 0:2, :]), r=[PT.b], w=[qdT.b])
            S_.act(lambda e: e.copy(out=qdT[64:128, :, 1, :], in_=PTv[64:128, 0:2, :]), r=[PT.b], w=[qdT.b])
            for h in range(2):
                S_.act(lambda e, h=h, t=t: e.copy(out=kc[h][:, t * 128:(t + 1) * 128], in_=PTv[:, 2 + h, :]), r=[PT.b], w=[kcb[h][t]])
            nblk = t + 1
            for h in range(2):
                accs = []
                for tt in range(2):
                    ps = slice(tt * 64, (tt + 1) * 64)
                    acc = P[2 + (acc_i[0] % 2)]
                    acc_i[0] += 1
                    accs.append(acc)
                    ngrp = (nblk + 3) // 4
                    for gi in range(ngrp):
                        j0 = gi * 4
                        nb = min(4, nblk - j0)
                        scb = P[4] if (gi % 2 == 0) else P[5]
                        scw = [scb.b] if scb is P[4] else [P5a, P5b]
                        pb = pbf[gi % 2]
                        for jj in range(nb):
                            j = j0 + jj
                            S_.pe(lambda e, jj=jj, j=j, tt=tt, h=h, scb=scb: e.matmul(
                                scb[:, jj * 128:(jj + 1) * 128], lhsT=kc[h][:, j * 128:(j + 1) * 128], rhs=qdT[:, h, tt, :],
                                start=True, stop=True), r=[kcb[h][j], qdT.b], w=scw)
                        S_.act(lambda e, nb=nb, scb=scb, pb=pb: e.activation(out=pb[:, 0:nb, :], in_=v3(scb[:, 0:nb * 128], nb),
                                                                            func=AF.Exp, scale=0.125), r=scw, w=[pb.b])
                        if j0 + nb - 1 == t:
                            jd = nb - 1
                            S_.pool(lambda e, jd=jd, pb=pb: e.tensor_tensor(out=pb[:, jd, :], in0=pb[:, jd, :], in1=maskTb, op=ALU.mult),
                                    r=[pb.b, cstb.b], w=[pb.b])
                        for jj in range(nb):
                            j = j0 + jj
                            S_.pe(lambda e, jj=jj, j=j, h=h, acc=acc, pb=pb, nblk=nblk: e.matmul(
                                acc[:, 0:130], lhsT=pb[:, jj, :], rhs=vc[h][:, j, :], start=(j == 0), stop=(j == nblk - 1)),
                                r=[pb.b, vcb[h][j]], w=[acc.b])
                a1, a2 = accs
                c0 = 24 + h * 2
                S_.dve(lambda e, a1=a1, c0=c0: e.reciprocal(out=stat[:, c0:c0 + 1], in_=a1[:, 128:129]), r=[a1.b], w=[stat.b])
                S_.dve(lambda e, a2=a2, c0=c0: e.reciprocal(out=stat[:, c0 + 1:c0 + 2], in_=a2[:, 128:129]), r=[a2.b], w=[stat.b])
                S_.dve(lambda e, c0=c0: e.tensor_tensor(out=stat[:, c0 + 1:c0 + 2], in0=stat[:, c0 + 1:c0 + 2], in1=small[:, 20:21], op=ALU.mult),
                       r=[stat.b, small.b], w=[stat.b])
                S_.act(lambda e, a1=a1, c0=c0, h=h: e.activation(out=f3[:, h * 128:(h + 1) * 128], in_=a1[:, 0:128], func=AF.Copy,
                                                                scale=stat[:, c0:c0 + 1]), r=[a1.b, stat.b], w=[f3.b])
                S_.dve(lambda e, a2=a2, c0=c0, h=h: e.scalar_tensor_tensor(out=f3[:, h * 128:(h + 1) * 128], in0=a2[:, 0:128],
                                                                          scalar=stat[:, c0 + 1:c0 + 2], in1=f3[:, h * 128:(h + 1) * 128],
                                                                          op0=ALU.mult, op1=ALU.add), r=[a2.b, stat.b, f3.b], w=[f3.b])
            S_.act(lambda e: e.activation(out=f1[:, 0:256], in_=f3[:, 0:256], func=AF.Square, scale=1.0 / math.sqrt(128.0)),
                   r=[f3.b], w=[f1.b])
            S_.dve(lambda e: e.tensor_reduce(out=stat[:, 28:30], in_=v3(f1[:, 0:256], 2), axis=AX.X, op=ALU.add), r=[f1.b], w=[stat.b])
            S_.dve(lambda e: e.tensor_scalar(out=stat[:, 28:30], in0=stat[:, 28:30], scalar1=EPS, scalar2=None, op0=ALU.add),
                   r=[stat.b], w=[stat.b])
            S_.act(lambda e: e.activation(out=stat[:, 28:30], in_=stat[:, 28:30], func=AF.Sqrt), r=[stat.b], w=[stat.b])
            S_.dve(lambda e: e.reciprocal(out=stat[:, 28:30], in_=stat[:, 28:30]), r=[stat.b], w=[stat.b])
            S_.dve(lambda e: e.tensor_tensor(out=v3(f3[:, 0:256], 2), in0=v3(f3[:, 0:256], 2), in1=bc_last(stat[:, 28:30], 128), op=ALU.mult),
                   r=[f3.b, stat.b], w=[f3.b])
            S_.pool(lambda e: e.tensor_tensor(out=v3(f3[:, 0:256], 2), in0=v3(f3[:, 0:256], 2), in1=bc_mid(par[:, PAR_DNW:PAR_DNW + 128], 2),
                                              op=ALU.mult), r=[f3.b, par.b], w=[f3.b])
            S_.pool(lambda e: e.tensor_tensor(out=mix[:, 256:512], in0=f3[:, 0:256], in1=zs[:, 256:512], op=ALU.mult),
                    r=[f3.b, zs.b], w=[mix.b])

            if marks is not None:
                marks.append((t, '---------- G: out-proj -----', len(S_.ops)))
            for k in range(8):
                S_.pe(lambda e, k=k: e.transpose(out=PTv[:, k, :], in_=mix[:, k * 128:(k + 1) * 128], identity=ident),
                      r=[mix.b, cstb.b], w=[PT.b])
            S_.act(lambda e: e.copy(out=mixT[:], in_=PTv), r=[PT.b], w=[mixT.b])
            for half in range(2):
                bank = P[half]
                for k in range(8):
                    S_.pe(lambda e, k=k, half=half, bank=bank: e.matmul(bank[:, 0:512], lhsT=mixT[:, k, :],
                                                                        rhs=wout[:, k, half * 512:(half + 1) * 512],
                                                                        start=(k == 0), stop=(k == 7)), r=[mixT.b, wout.b], w=[bank.b])
                if half == 0:
                    S_.act(lambda e, bank=bank: e.copy(out=f1[:], in_=bank[:, 0:512]), r=[bank.b], w=[f1.b])
                else:
                    S_.dve(lambda e, bank=bank: e.tensor_copy(out=f2[:], in_=bank[:, 0:512]), r=[bank.b], w=[f2.b])
            ob = Buf("outd")
            obs.append(ob)
            S_.dma("sp", lambda e, tok=tok: e.dma_start(out=out_d[tok, 0:512], in_=f1[:]), r=[f1.b], w=[ob])
            ob = Buf("outd")
            obs.append(ob)
            S_.dma("sp", lambda e, tok=tok: e.dma_start(out=out_d[tok, 512:1024], in_=f2[:]), r=[f2.b], w=[ob])
            if debug:
                for q in range(2):
                    S_.act(lambda e, q=q: e.copy(out=f3[:], in_=mix[:, q * 512:(q + 1) * 512]), r=[mix.b], w=[f3.b])
                    ob = Buf("dbgd")
                    obs.append(ob)
                    S_.dma("sp", lambda e, tok=tok, q=q: e.dma_start(out=dbg_d[tok, q * 512:(q + 1) * 512], in_=f3[:]), r=[f3.b], w=[ob])
        if trunc is not None:
            S_.ops = S_.ops[:trunc]
        else:
            S_.add("sp", None, reads=obs)
        S_.emit(st)
    return nc


def build_final(ntok, nparts):
    nc = bass.Bass("TRN2", target_bir_lowering=False)
    dr = lambda n, shp, kind="ExternalInput": nc.dram_tensor(n, shp, F32, kind=kind).ap()
    ins = [dr("p%d" % i, [ntok, D_MODEL]) for i in range(nparts)]
    w_d = dr("w", [1, D_MODEL])
    out_d = dr("out", [ntok, D_MODEL], kind="ExternalOutput")
    with ExitStack() as st:
        S_ = Sched(nc)
        sb = lambda n, shp, dt=F32: T(st, nc, n, shp, dt)
        wt = sb("wt", [128, D_MODEL])
        xs = [[sb("x%d_%d" % (i, q), [128, D_MODEL]) for i in range(nparts)] for q in range(2)]
        junk = sb("junk", [128, D_MODEL])
        stat = sb("stat", [128, 4])
        S_.dma("sp", lambda e: e.dma_start(out=wt[:], in_=w_d[0:1, :].broadcast_to([128, D_MODEL])), w=[wt.b])
        ob = Buf("o")
        for t in range(ntok // 128):
            tok = slice(t * 128, (t + 1) * 128)
            xx = xs[t % 2]
            for i in range(nparts):
                S_.dma("sp", lambda e, i=i, xx=xx, tok=tok: e.dma_start(out=xx[i][:], in_=ins[i][tok, :]), w=[xx[i].b])
            for i in range(1, nparts):
                eng = S_.pool if i % 2 else S_.dve
                eng(lambda e, i=i, xx=xx: e.tensor_tensor(out=xx[0][:], in0=xx[0][:], in1=xx[i][:], op=ALU.add),
                    r=[xx[0].b, xx[i].b], w=[xx[0].b])
            S_.act(lambda e, xx=xx: e.activation(out=junk[:], in_=xx[0][:], func=AF.Square, scale=1.0 / 32.0, accum_out=stat[:, 0:1]),
                   r=[xx[0].b], w=[junk.b, stat.b])
            S_.dve(lambda e: e.tensor_scalar(out=stat[:, 0:1], in0=stat[:, 0:1], scalar1=EPS, scalar2=None, op0=ALU.add),
                   r=[stat.b], w=[stat.b])
            S_.act(lambda e: e.activation(out=stat[:, 1:2], in_=stat[:, 0:1], func=AF.Sqrt), r=[stat.b], w=[stat.b])
            S_.dve(lambda e: e.reciprocal(out=stat[:, 1:2], in_=stat[:, 1:2]), r=[stat.b], w=[stat.b])
            S_.dve(lambda e, xx=xx: e.scalar_tensor_tensor(out=xx[1][:], in0=xx[0][:], scalar=stat[:, 1:2], in1=wt[:],
                                                           op0=ALU.mult, op1=ALU.mult), r=[xx[0].b, stat.b, wt.b], w=[xx[1].b])
            S_.dma("sp", lambda e, xx=xx, tok=tok: e.dma_start(out=out_d[tok, :], in_=xx[1][:]), r=[xx[1].b], w=[ob])
        S_.add("sp", None, reads=[ob])
        S_.emit(st)
    return nc


def layer_inputs(l, j, S, p):
    tm, fm, orow = _col_index(j)
    hs = (2 * j, 2 * j + 1)
    gs = (j, 1 - j)
    w_in = p["w_in"][l]
    d = {}
    d["wtm"] = np.ascontiguousarray(w_in[:, tm])
    d["wfm"] = np.ascontiguousarray(w_in[:, fm])
    d["wout"] = np.ascontiguousarray(p["w_out"][l][orow, :])
    d["rope"] = _rope(S)
    d["cst"] = _consts(j)
    d["nwfm"] = np.ascontiguousarray(p["norm_w"][l].reshape(8, 128).T)
    par = np.zeros((1, NPAR), np.float32)
    par[0, 0:128] = p["diff_norm_w"][l]
    par[0, 128:384] = p["ssd_norm_w"][l][j * 256:(j + 1) * 256]
    par[0, 384:640] = p["mlstm_norm_w"][l][2 * j * 128:(2 * j + 2) * 128]
    gi = np.concatenate([g * 4 + np.arange(4) for g in gs])
    par[0, 640:648] = p["ssd_dt_bias"][l][gi]
    par[0, 648:650] = p["mlstm_gate_b"][l][4 + np.array(hs)]
    par[0, 650:652] = p["mlstm_gate_b"][l][np.array(hs)]
    par[0, 652:660] = p["ssd_a_log"][l][gi]
    par[0, 660:668] = p["ssd_d"][l][gi]
    par[0, 668:924] = p["diff_lambda"][l].reshape(-1)
    d["par"] = par
    xbc_ch = np.concatenate([g * 256 + np.arange(256) for g in gs] + [512 + g * 128 + np.arange(128) for g in gs]
                            + [768 + g * 128 + np.arange(128) for g in gs])
    qk_ch = np.concatenate([h * 128 + np.arange(128) for h in hs] + [512 + h * 128 + np.arange(128) for h in hs])
    cw = np.concatenate([p["ssd_conv_w"][l][:, xbc_ch], p["mlstm_conv_w"][l][:, qk_ch]], axis=1)
    cb = np.concatenate([p["ssd_conv_b"][l][xbc_ch], p["mlstm_conv_b"][l][qk_ch]])[None, :]
    c5 = np.concatenate([cw, cb], axis=0)
    d["convw"] = np.ascontiguousarray(c5.reshape(5, 12, 128).transpose(2, 1, 0).reshape(128, 60))
    return d


_PROG_CACHE = {}


def _prog(key, fn):
    if key not in _PROG_CACHE:
        _PROG_CACHE[key] = fn()
    return _PROG_CACHE[key]


def kernel(x, norm_w, w_in, w_out, diff_lambda, diff_norm_w, ssd_conv_w, ssd_conv_b, ssd_dt_bias,
           ssd_a_log, ssd_d, ssd_norm_w, mlstm_conv_w, mlstm_conv_b, mlstm_gate_b, mlstm_norm_w, final_norm_w):
    p = dict(norm_w=norm_w, w_in=w_in, w_out=w_out, diff_lambda=diff_lambda, diff_norm_w=diff_norm_w,
             ssd_conv_w=ssd_conv_w, ssd_conv_b=ssd_conv_b, ssd_dt_bias=ssd_dt_bias, ssd_a_log=ssd_a_log,
             ssd_d=ssd_d, ssd_norm_w=ssd_norm_w, mlstm_conv_w=mlstm_conv_w, mlstm_conv_b=mlstm_conv_b,
             mlstm_gate_b=mlstm_gate_b, mlstm_norm_w=mlstm_norm_w)
    p = {k: np.asarray(v, np.float32) for k, v in p.items()}
    x = np.asarray(x, np.float32)
    Bn, S, _ = x.shape
    depth = w_in.shape[0]
    ncores = 2 * Bn
    partials = []
    for l in range(depth):
        lam_init = 0.8 - 0.6 * math.exp(-0.3 * l)
        nprev = 2 * l
        nc = build_layer(S, nprev, lam_init)
        in_maps = []
        for c in range(ncores):
            b, j = c // 2, c % 2
            d = layer_inputs(l, j, S, p)
            d["x"] = np.ascontiguousarray(x[b])
            i = 0
            for pl in partials:
                for jj in range(2):
                    d["prev%d" % i] = pl[2 * b + jj]
                    i += 1
            in_maps.append(d)
        res = run_bass_kernel_spmd(nc, in_maps, core_ids=list(range(ncores)))
        partials.append([res.results[c]["out"] for c in range(ncores)])
    ntok = Bn * S // ncores
    nparts = 1 + 2 * depth
    nc = build_final(ntok, nparts)
    in_maps = []
    for c in range(ncores):
        b = (c * ntok) // S
        o = (c * ntok) % S
        d = {"p0": np.ascontiguousarray(x[b, o:o + ntok]), "w": np.asarray(final_norm_w, np.float32).reshape(1, -1)}
        i = 1
        for pl in partials:
            for jj in range(2):
                d["p%d" % i] = np.ascontiguousarray(pl[2 * b + jj][o:o + ntok])
                i += 1
        in_maps.append(d)
    res = run_bass_kernel_spmd(nc, in_maps, core_ids=list(range(ncores)))
    out = np.concatenate([res.results[c]["out"] for c in range(ncores)], axis=0).reshape(Bn, S, D_MODEL)
    return out.astype(np.float32)
```

```python
import math
from contextlib import ExitStack

import numpy as np
import concourse.bass as bass
import concourse.mybir as mybir
from concourse.bass_utils import run_bass_kernel_spmd

F32 = mybir.dt.float32
BF16 = mybir.dt.bfloat16
ALU = mybir.AluOpType
AF = mybir.ActivationFunctionType
AX = mybir.AxisListType

D_MODEL = 1024
CH = 128
EPS = 1e-6
RET_B, DIFF_B, SSD_B, ML_B = 0, 1536, 3584, 5128
NTM = 3084
NFM = 1536
NPAR = 924


class Buf:
    __slots__ = ("name", "last_w", "readers", "excl", "last_acc")

    def __init__(self, name="", excl=False):
        self.name = name
        self.last_w = None
        self.readers = []
        self.excl = excl
        self.last_acc = None


class Op:
    __slots__ = ("eng", "fn", "reads", "writes", "dma", "deps", "inc", "tick", "sem", "idx", "waits")

    def __init__(self, eng, fn, reads, writes, dma):
        self.eng = eng
        self.fn = fn
        self.reads = reads
        self.writes = writes
        self.dma = dma
        self.deps = []
        self.inc = False
        self.tick = None
        self.sem = None
        self.waits = []


class Sched:
    ENGS = ("pe", "act", "dve", "pool", "sp")
    DMA_RING = 8

    def __init__(self, nc):
        self.nc = nc
        self.ops = []
        self.cur = self.ops

    def add(self, eng, fn, reads=(), writes=(), dma=False):
        op = Op(eng, fn, tuple(reads), tuple(writes), dma)
        self.cur.append(op)
        return op

    def capture(self, lst):
        self.cur = self.ops if lst is None else lst

    def merge(self, a, b):
        out = []
        ia = ib = 0
        while ia < len(a) or ib < len(b):
            if ib >= len(b) or (ia < len(a) and ia * len(b) <= ib * len(a)):
                out.append(a[ia])
                ia += 1
            else:
                out.append(b[ib])
                ib += 1
        self.ops.extend(out)

    def pe(self, fn, r=(), w=()):
        return self.add("pe", fn, r, w)

    def act(self, fn, r=(), w=()):
        return self.add("act", fn, r, w)

    def dve(self, fn, r=(), w=()):
        return self.add("dve", fn, r, w)

    POOL_TO = "pool"

    def pool(self, fn, r=(), w=()):
        return self.add(self.POOL_TO, fn, r, w)

    def dma(self, eng, fn, r=(), w=()):
        return self.add(eng, fn, r, w, dma=True)

    def resolve(self, stack):
        ops = self.ops
        for i, op in enumerate(ops):
            op.idx = i
        for op in ops:
            deps = {}
            for b in op.reads:
                if b.last_w is not None:
                    deps[b.last_w] = "raw"
            for b in op.writes:
                if b.last_w is not None and b.last_w not in deps:
                    deps[b.last_w] = "waw"
                for r in b.readers:
                    if r not in deps and r != op.idx:
                        deps[r] = "war"
            for b in op.reads + op.writes:
                if b.excl:
                    if b.last_acc is not None and ops[b.last_acc].eng != op.eng:
                        deps[b.last_acc] = "raw"
                    b.last_acc = op.idx
            for b in op.reads:
                b.readers.append(op.idx)
            for b in op.writes:
                b.last_w = op.idx
                b.readers = []
            for pi, kind in deps.items():
                p = ops[pi]
                if (not p.dma) and (not op.dma) and p.eng == op.eng:
                    if op.eng == "pe":
                        continue
                    if kind != "raw":
                        continue
                op.deps.append(pi)
                p.inc = True
        self.eng_sem = {}
        self.dma_sems = {}
        for e in self.ENGS:
            self.eng_sem[e] = stack.enter_context(self.nc.semaphore("tick_" + e))
        for e in ("sp", "pool", "act"):
            self.dma_sems[e] = [stack.enter_context(self.nc.semaphore("dma_%s_%d" % (e, i)))
                                for i in range(self.DMA_RING)]
        tick = {e: 0 for e in self.ENGS}
        dcount = {e: 0 for e in self.ENGS}
        for op in ops:
            if op.dma:
                n = dcount[op.eng]
                dcount[op.eng] += 1
                op.sem = self.dma_sems[op.eng][n % self.DMA_RING]
                op.tick = 16 * (n // self.DMA_RING + 1)
                op.inc = True
                if n >= self.DMA_RING:
                    op.waits.append((op.sem, op.tick - 16))
            elif op.inc:
                tick[op.eng] += 1
                op.sem = self.eng_sem[op.eng]
                op.tick = tick[op.eng]
        seen = {e: {} for e in self.ENGS}
        for op in ops:
            s = seen[op.eng]
            need = {}
            for (sem, val) in op.waits:
                need[id(sem)] = (sem, val)
            for pi in op.deps:
                p = ops[pi]
                k = id(p.sem)
                if k not in need or need[k][1] < p.tick:
                    need[k] = (p.sem, p.tick)
            op.waits = []
            for k, (sem, val) in need.items():
                if s.get(k, 0) >= val:
                    continue
                s[k] = val
                op.waits.append((sem, val))

    def emit(self, stack):
        self.resolve(stack)
        nc = self.nc
        per = {e: [op for op in self.ops if op.eng == e] for e in self.ENGS}
        block = stack.enter_context(nc.Block())

        def run(eng_handle, lst):
            for op in lst:
                for (sem, val) in op.waits:
                    eng_handle.wait_ge(sem, val)
                if op.fn is None:
                    continue
                ins = op.fn(eng_handle)
                if op.inc:
                    ins.then_inc(op.sem, 16 if op.dma else 1)

        @block.tensor
        def _(e):
            run(e, per["pe"])

        @block.scalar
        def _(e):
            run(e, per["act"])

        @block.vector
        def _(e):
            run(e, per["dve"])

        @block.gpsimd
        def _(e):
            run(e, per["pool"])

        @block.sync
        def _(e):
            run(e, per["sp"])


class T:
    def __init__(self, st, nc, name, shape, dt, psum=False):
        alloc = nc.psum_tensor if psum else nc.sbuf_tensor
        self.t = st.enter_context(alloc(name, shape, dt))
        self.b = Buf(name, excl=psum)

    def __getitem__(self, k):
        return self.t[k]


def _col_index(j):
    hs = (2 * j, 2 * j + 1)
    gs = (j, 1 - j)
    r = np.arange
    tm = []
    tm += [RET_B + h * 64 + r(64) for h in hs]
    tm += [RET_B + 256 + h * 64 + r(64) for h in hs]
    tm += [DIFF_B + h * 128 + r(128) for h in hs]
    tm += [DIFF_B + 512 + h * 128 + r(128) for h in hs]
    tm += [RET_B + 512 + h * 128 + r(128) for h in hs]
    tm += [DIFF_B + 1024 + h * 128 + r(128) for h in hs]
    tm += [ML_B + 1024 + h * 128 + r(128) for h in hs]
    tm += [RET_B + 1024 + h * 128 + r(128) for h in hs]
    tm += [DIFF_B + 1536 + h * 128 + r(128) for h in hs]
    tm += [SSD_B + 1032 + g * 256 + r(256) for g in gs]
    tm += [ML_B + 2056 + h * 128 + r(128) for h in hs]
    tm += [ML_B + 1536 + h * 128 + r(128) for h in hs]
    tm += [SSD_B + 1024 + g * 4 + r(4) for g in gs]
    tm += [ML_B + 2048 + 4 + np.array(hs)]
    tm += [ML_B + 2048 + np.array(hs)]
    tm = np.concatenate(tm)
    fm = []
    fm += [SSD_B + g * 256 + r(256) for g in gs]
    fm += [SSD_B + 512 + g * 128 + r(128) for g in gs]
    fm += [SSD_B + 768 + g * 128 + r(128) for g in gs]
    fm += [ML_B + h * 128 + r(128) for h in hs]
    fm += [ML_B + 512 + h * 128 + r(128) for h in hs]
    fm = np.concatenate(fm)
    assert tm.size == NTM and fm.size == NFM
    orow = np.concatenate([h * 128 + r(128) for h in hs] + [512 + h * 128 + r(128) for h in hs]
                          + [1024 + j * 256 + r(256)] + [1536 + h * 128 + r(128) for h in hs])
    return tm, fm, orow


def _consts(j):
    c = np.zeros((128, 392), np.float32)
    c2 = np.zeros((128, 384), np.float32)
    s = np.arange(128)[:, None]
    l = np.arange(128)[None, :]
    c[:, 0:128] = (l >= s)
    c2[:, 0:128] = (s > l)
    for hh in range(2):
        h = 2 * j + hh
        lg = math.log(1.0 - 2.0 ** (-5.0 - h))
        c[:, 128 + hh * 128:128 + (hh + 1) * 128] = np.where(l >= s, np.exp(lg * np.maximum(l - s, 0)), 0.0) * 0.125
        c[:, 384 + hh] = np.exp(lg * (np.arange(128) + 1.0))
        c[:, 386 + hh] = np.exp(lg * (127.0 - np.arange(128))) * 0.125
        c[hh * 64:(hh + 1) * 64, 388] = np.exp(lg * 128.0)
    c2[:, 128:256] = np.eye(128)
    c2[:, 256:384] = 1.0
    return c, c2


def _rope(S):
    inv = (10000.0 ** (-np.arange(0, 64, 2, dtype=np.float32) / np.float32(64))).astype(np.float32)
    ang = np.arange(S, dtype=np.float32)[:, None] * inv[None, :]
    cos, sin = np.cos(ang).astype(np.float32), np.sin(ang).astype(np.float32)
    return np.concatenate([cos, cos, -sin, sin], axis=1).astype(np.float32)


def build_layer(S, nprev, lam_init, debug=False, trunc=None, marks=None, noweights=False):
    NCH = S // CH
    nc = bass.Bass("TRN2", target_bir_lowering=False)
    dr = lambda n, shp, kind="ExternalInput": nc.dram_tensor(n, shp, F32, kind=kind).ap()
    x_d = dr("x", [S, D_MODEL])
    prev_d = [dr("prev%d" % i, [S, D_MODEL]) for i in range(nprev)]
    wtm_d = dr("wtm", [D_MODEL, NTM])
    wfm_d = dr("wfm", [D_MODEL, NFM])
    wout_d = dr("wout", [D_MODEL, D_MODEL])
    rope_d = dr("rope", [S, 128])
    par_d = dr("par", [1, NPAR])
    nwfm_d = dr("nwfm", [128, 8])
    convw_d = dr("convw", [128, 60])
    cst_d = dr("cst", [128, 392])
    cst2_d = dr("cst2", [128, 384])
    out_d = dr("out", [S, D_MODEL], kind="ExternalOutput")
    dbg_d = dr("dbg", [S, D_MODEL], kind="ExternalOutput") if debug else None

    with ExitStack() as st:
        S_ = Sched(nc)
        sb = lambda n, shp, dt=F32: T(st, nc, n, shp, dt)
        wtm = sb("wtm_s", [128, 8, NTM], BF16)
        wfm = sb("wfm_s", [128, 8, NFM], BF16)
        wout = sb("wout_s", [128, 8, D_MODEL], BF16)
        kc = [sb("kc%d" % h, [128, S], BF16) for h in range(2)]
        vc = [sb("vc%d" % h, [128, NCH, 130], BF16) for h in range(2)]
        kcb = [[Buf() for _ in range(NCH)] for _ in range(2)]
        vcb = [[Buf() for _ in range(NCH)] for _ in range(2)]
        cst = sb("cst_s", [128, 392])
        cstb = sb("cstb_s", [128, 512], BF16)
        gbb = sb("gbb", [128, 2, 4, 128], BF16)
        hl = sb("hl", [128, 2, 16], BF16)
        par = sb("par_s", [128, 668])
        nwfm = sb("nwfm_s", [128, 8])
        convw = sb("convw_s", [128, 60])
        small = sb("small_s", [128, 24])
        rst = sb("rst", [128, 128])
        rstb = sb("rstb", [128, 128], BF16)
        sst = sb("sst", [128, 512])
        sstb = sb("sstb", [128, 512], BF16)
        mst = sb("mst", [128, 2, 130])
        mstb = sb("mstb", [128, 2, 130], BF16)
        rawfm = sb("rawfm", [128, 12, 131])
        mlv = sb("mlv", [128, 2, 130], BF16)
        hin = [sb("hin0", [128, D_MODEL])] * 2
        ropet = [sb("rope0", [128, 128])] * 2
        uT = sb("uT", [128, 8, 128], BF16)
        stat = sb("stat", [128, 32])
        f1 = sb("f1", [128, 512])
        f2 = sb("f2", [128, 512])
        f3 = sb("f3", [128, 512])
        qk0 = sb("qk0", [128, 512], BF16)
        kd = sb("kd", [128, 256], BF16)
        rqg = sb("rqg", [128, 128], BF16)
        rkg = sb("rkg", [128, 128], BF16)
        rv = sb("rv", [128, 256], BF16)
        rTk = sb("rTk", [128, 128], BF16)
        rTm = sb("rTm", [128, 4, 128], BF16)
        qdT = sb("qdT", [128, 2, 2, 128], BF16)
        zs = sb("zs", [128, 1280], BF16)
        sgo = sb("sgo", [128, 256], BF16)
        g6 = sb("g6", [128, 16])
        g6b = sb("g6b", [128, 16])
        dA = sb("dA", [128, 8])
        class _View:
            def __init__(self, base, view):
                self.b = base.b
                self.v = view

            def __getitem__(self, k):
                return self.v[k]
        fmc = _View(f3, f3[:].rearrange("p (a b) -> p a b", a=4))
        fmb = sb("fmb", [128, 12, 128], BF16)
        b1 = sb("b1", [128, 1024], BF16)
        b2 = sb("b2", [128, 1024], BF16)
        b3 = sb("b3", [128, 1024], BF16)
        pbf = [sb("pbf%d" % i, [128, 4, 128], BF16) for i in range(2)]
        mix = sb("mix", [128, D_MODEL], BF16)
        fd = sb("fd", [128, 512])
        statd = sb("statd", [128, 8])
        mixT = uT
        ubf = b3
        P = [T(st, nc, "P%d" % i, [128, 512], F32, psum=True) for i in range(7)]
        PT = T(st, nc, "PT", [128, 1024], BF16, psum=True)
        PTv = PT[:].rearrange("p (a b) -> p a b", a=8)

        maskT = cst[:, 0:128]
        ident = cstb[:, 128:256]
        maskTb = cstb[:, 0:128]
        Lstb = cstb[:, 256:384]
        onesb = cstb[:, 384:512]

        def v3(ap, a):
            return ap.rearrange("p (a b) -> p a b", a=a)

        def bc_last(ap2, n):
            return ap2.unsqueeze(2).broadcast_to([ap2.shape[0], ap2.shape[1], n])

        def bc_mid(ap2, a):
            return ap2.unsqueeze(1).broadcast_to([ap2.shape[0], a, ap2.shape[1]])

        S_.dma("sp", lambda e: e.dma_start(out=cst[:], in_=cst_d[:, :]), w=[cst.b])
        S_.dma("sp", lambda e: e.dma_start(out=par[:], in_=par_d[0:1, 0:668].broadcast_to([128, 668])), w=[par.b])
        S_.dma("sp", lambda e: e.dma_start(out=nwfm[:], in_=nwfm_d[:, :]), w=[nwfm.b])
        S_.dma("sp", lambda e: e.dma_start(out=convw[:], in_=convw_d[:, :]), w=[convw.b])
        stg = [f1, f2, f3]
        cnt = [0]

        def load_cast(dst, src_d, ncols):
            for k in range(0 if noweights else 8):
                for c0 in range(0, ncols, 512):
                    w_ = min(512, ncols - c0)
                    sg = stg[cnt[0] % 3]
                    ci = cnt[0] % 3
                    ce = (S_.dve, S_.act, S_.pool)[ci]
                    cnt[0] += 1
                    S_.dma("sp", lambda e, sg=sg, k=k, c0=c0, w_=w_: e.dma_start(out=sg[:, 0:w_], in_=src_d[k * 128:(k + 1) * 128, c0:c0 + w_]),
                           w=[sg.b])
                    if ci == 1:
                        ce(lambda e, sg=sg, k=k, c0=c0, w_=w_: e.copy(out=dst[:, k, c0:c0 + w_], in_=sg[:, 0:w_]), r=[sg.b], w=[dst.b])
                    else:
                        ce(lambda e, sg=sg, k=k, c0=c0, w_=w_: e.tensor_copy(out=dst[:, k, c0:c0 + w_], in_=sg[:, 0:w_]), r=[sg.b], w=[dst.b])

        load_cast(wtm, wtm_d, NTM)
        load_cast(wfm, wfm_d, NFM)
        load_cast(wout, wout_d, D_MODEL)
        S_.dve(lambda e: e.tensor_copy(out=cstb[:, 0:128], in_=cst[:, 0:128]), r=[cst.b], w=[cstb.b])
        S_.dma("sp", lambda e: e.dma_start(out=f1[:, 0:384], in_=cst2_d[:, :]), w=[f1.b])
        S_.dve(lambda e: e.tensor_copy(out=cstb[:, 128:256], in_=f1[:, 128:256]), r=[f1.b], w=[cstb.b])
        S_.dve(lambda e: e.tensor_copy(out=cstb[:, 256:384], in_=f1[:, 0:128]), r=[f1.b], w=[cstb.b])
        S_.dve(lambda e: e.tensor_copy(out=cstb[:, 384:512], in_=f1[:, 256:384]), r=[f1.b], w=[cstb.b])
        for t_ in (rst, sst, mst, rawfm):
            S_.pool(lambda e, t_=t_: e.memset(t_[:], 0.0), w=[t_.b])
        for t_ in (rstb, sstb, mstb):
            S_.pool(lambda e, t_=t_: e.memset(t_[:], 0.0), w=[t_.b])
        S_.pool(lambda e: e.memset(mlv[:], 1.0), w=[mlv.b])
        S_.pool(lambda e: e.memset(rTm[:], 0.0), w=[rTm.b])
        S_.pool(lambda e: e.memset(qdT[:], 0.0), w=[qdT.b])
        for h in range(2):
            S_.pool(lambda e, h=h: e.memset(vc[h][:], 1.0), w=[vc[h].b] + vcb[h])
        PAR_DNW, PAR_SNW, PAR_MNW, PAR_B12, PAR_AL, PAR_DS, PAR_LAM = 0, 128, 384, 640, 652, 660, 668
        S_.act(lambda e: e.activation(out=small[:, 0:8], in_=par[:, PAR_AL:PAR_AL + 8], func=AF.Exp), r=[par.b], w=[small.b])
        S_.dve(lambda e: e.tensor_scalar(out=small[:, 0:8], in0=small[:, 0:8], scalar1=-1.0, scalar2=None, op0=ALU.mult),
               r=[small.b], w=[small.b])
        S_.dma("sp", lambda e: e.dma_start(out=f2[:, 0:256], in_=par_d[0:1, 668:924].broadcast_to([128, 256])), w=[f2.b])
        S_.dve(lambda e: e.tensor_tensor(out=f1[:, 0:128].rearrange("p (a b) -> p a b", a=2),
                                         in0=f2[:, 0:256].rearrange("p (a t b) -> p a t b", a=2, t=2)[:, :, 0, :],
                                         in1=f2[:, 0:256].rearrange("p (a t b) -> p a t b", a=2, t=2)[:, :, 1, :],
                                         op=ALU.mult), r=[f2.b], w=[f1.b])
        S_.dve(lambda e: e.tensor_reduce(out=small[:, 16:18], in_=f1[:, 0:128].rearrange("p (a b) -> p a b", a=2),
                                         axis=AX.X, op=ALU.add), r=[f1.b], w=[small.b])
        S_.act(lambda e: e.activation(out=small[:, 18:20], in_=small[:, 16:18], func=AF.Exp), r=[small.b], w=[small.b])
        S_.dve(lambda e: e.scalar_tensor_tensor(out=small[:, 20:21], in0=small[:, 19:20], scalar=-float(lam_init),
                                                in1=small[:, 18:19], op0=ALU.add, op1=ALU.subtract),
               r=[small.b], w=[small.b])
        S_.dve(lambda e: e.tensor_scalar(out=par[:, PAR_DNW:PAR_DNW + 128], in0=par[:, PAR_DNW:PAR_DNW + 128],
                                         scalar1=float(1.0 - lam_init), scalar2=None, op0=ALU.mult), r=[par.b], w=[par.b])

        R = {"P5a": P[5].b, "P5b": P[5].b, "P6a": P[6].b, "P6b": P[6].b}
        P5a, P5b, P6a, P6b = R["P5a"], R["P5b"], R["P6a"], R["P6b"]
        acc_i = [0]
        obs = []

        def rms_tail(src_ap3, nh, n, eps_in, dst_stat_col):
            c = dst_stat_col
            return c

        for t in range(NCH):
            hb = hin[t % 2]
            rp = ropet[t % 2]
            tok = slice(t * CH, (t + 1) * CH)
            if marks is not None:
                marks.append((t, '---------- A: load + rmsnorm', len(S_.ops)))
            S_.dma("sp", lambda e, hb=hb, tok=tok: e.dma_start(out=hb[:], in_=x_d[tok, :]), w=[hb.b])
            S_.dma("sp", lambda e, rp=rp, tok=tok: e.dma_start(out=rp[:], in_=rope_d[tok, :]), w=[rp.b])
            for i in range(nprev):
                for hf, sg in enumerate((f1, f2)):
                    S_.dma("sp", lambda e, i=i, tok=tok, hf=hf, sg=sg: e.dma_start(out=sg[:], in_=prev_d[i][tok, hf * 512:(hf + 1) * 512]),
                           w=[sg.b])
                    S_.pool(lambda e, hb=hb, hf=hf, sg=sg: e.tensor_tensor(out=hb[:, hf * 512:(hf + 1) * 512], in0=hb[:, hf * 512:(hf + 1) * 512],
                                                                           in1=sg[:], op=ALU.add), r=[hb.b, sg.b], w=[hb.b])
            S_.act(lambda e, hb=hb: e.activation(out=b2[:], in_=hb[:], func=AF.Square, scale=1.0 / 32.0,
                                                 accum_out=stat[:, 0:1]), r=[hb.b], w=[b2.b, stat.b])
            S_.dve(lambda e: e.tensor_scalar(out=stat[:, 0:1], in0=stat[:, 0:1], scalar1=EPS, scalar2=None, op0=ALU.add),
                   r=[stat.b], w=[stat.b])
            S_.act(lambda e: e.activation(out=stat[:, 1:2], in_=stat[:, 0:1], func=AF.Sqrt), r=[stat.b], w=[stat.b])
            S_.dve(lambda e: e.reciprocal(out=stat[:, 1:2], in_=stat[:, 1:2]), r=[stat.b], w=[stat.b])
            S_.dve(lambda e, hb=hb: e.tensor_scalar(out=ubf[:], in0=hb[:], scalar1=stat[:, 1:2], scalar2=None, op0=ALU.mult),
                   r=[hb.b, stat.b], w=[ubf.b])
            for k in range(8):
                S_.pe(lambda e, k=k: e.transpose(out=PTv[:, k, :], in_=ubf[:, k * 128:(k + 1) * 128], identity=ident),
                      r=[ubf.b, cstb.b], w=[PT.b])
            S_.dve(lambda e: e.tensor_tensor(out=uT[:], in0=PTv, in1=bc_last(nwfm[:, 0:8], 128), op=ALU.mult),
                   r=[PT.b, nwfm.b], w=[uT.b])

            if marks is not None:
                marks.append((t, '---------- B: in-proj ------', len(S_.ops)))
            def tm_group(g, width, bank):
                for k in range(8):
                    S_.pe(lambda e, k=k: e.matmul(bank[:, 0:width], lhsT=uT[:, k, :], rhs=wtm[:, k, g * 512:g * 512 + width],
                                                  start=(k == 0), stop=(k == 7)), r=[uT.b, wtm.b], w=[bank.b])

            def rope(bank, ncols, dst_ap, extra_w):
                nh = ncols // 64
                src4 = bank[:, 0:ncols].rearrange("p (h t d) -> p h t d", h=nh, t=2)
                S_.dve(lambda e: e.tensor_tensor(out=v3(f1[:, 0:ncols], nh), in0=v3(bank[:, 0:ncols], nh),
                                                 in1=bc_mid(rp[:, 0:64], nh), op=ALU.mult), r=[bank.b, rp.b], w=[f1.b])
                f24 = f2[:, 0:ncols].rearrange("p (h t d) -> p h t d", h=nh, t=2)
                S_.dve(lambda e: e.tensor_tensor(out=f24[:, :, 0, :], in0=src4[:, :, 1, :],
                                                 in1=bc_mid(rp[:, 64:96], nh), op=ALU.mult), r=[bank.b, rp.b], w=[f2.b])
                S_.dve(lambda e: e.tensor_tensor(out=f24[:, :, 1, :], in0=src4[:, :, 0, :],
                                                 in1=bc_mid(rp[:, 96:128], nh), op=ALU.mult), r=[bank.b, rp.b], w=[f2.b])
                S_.pool(lambda e: e.tensor_tensor(out=dst_ap, in0=f1[:, 0:ncols], in1=f2[:, 0:ncols], op=ALU.add),
                        r=[f1.b, f2.b], w=extra_w)

            tm_group(0, 512, P[0])
            rope(P[0], 512, qk0[:], [qk0.b])
            tm_group(1, 512, P[1])
            rope(P[1], 256, kd[:], [kd.b])
            S_.act(lambda e: e.copy(out=rv[:], in_=P[1][:, 256:512]), r=[P[1].b], w=[rv.b])
            tm_group(2, 512, P[0])
            for h in range(2):
                S_.act(lambda e, h=h, t=t: e.copy(out=vc[h][:, t, 0:128], in_=P[0][:, h * 128:(h + 1) * 128]),
                       r=[P[0].b], w=[vcb[h][t]])
            S_.act(lambda e: e.copy(out=mlv[:, :, 0:128], in_=v3(P[0][:, 256:512], 2)), r=[P[0].b], w=[mlv.b])
            tm_group(3, 512, P[1])
            S_.act(lambda e: e.activation(out=zs[:, 0:512], in_=P[1][:, 0:512], func=AF.Silu), r=[P[1].b], w=[zs.b])
            tm_group(4, 512, P[0])
            S_.act(lambda e: e.activation(out=zs[:, 512:1024], in_=P[0][:, 0:512], func=AF.Silu), r=[P[0].b], w=[zs.b])
            tm_group(5, 512, P[1])
            S_.act(lambda e: e.activation(out=zs[:, 1024:1280], in_=P[1][:, 0:256], func=AF.Silu), r=[P[1].b], w=[zs.b])
            S_.act(lambda e: e.activation(out=sgo[:], in_=P[1][:, 256:512], func=AF.Sigmoid), r=[P[1].b], w=[sgo.b])
            tm_group(6, 12, P[0])
            S_.dve(lambda e: e.tensor_tensor(out=g6[:, 0:12], in0=P[0][:, 0:12], in1=par[:, PAR_B12:PAR_B12 + 12], op=ALU.add),
                   r=[P[0].b, par.b], w=[g6.b])
            S_.act(lambda e: e.activation(out=g6b[:, 0:8], in_=g6[:, 0:8], func=AF.Exp), r=[g6.b], w=[g6b.b])
            S_.act(lambda e: e.activation(out=g6b[:, 8:10], in_=g6[:, 8:10], func=AF.Exp, scale=-1.0), r=[g6.b], w=[g6b.b])
            S_.act(lambda e: e.activation(out=g6b[:, 0:10], in_=g6b[:, 0:10], func=AF.Ln, bias=1.0), r=[g6b.b], w=[g6b.b])
            S_.dve(lambda e: e.tensor_tensor(out=dA[:], in0=g6b[:, 0:8], in1=small[:, 0:8], op=ALU.mult),
                   r=[g6b.b, small.b], w=[dA.b])
            S_.dve(lambda e: e.tensor_scalar(out=g6b[:, 8:10], in0=g6b[:, 8:10], scalar1=-1.0, scalar2=None, op0=ALU.mult),
                   r=[g6b.b], w=[g6b.b])
            if marks is not None:
                marks.append((t, 'FM blocks (+ depthwise', len(S_.ops)))
            for q4 in range(3):
                bank = P[1] if q4 % 2 == 0 else P[0]
                for bb in range(4):
                    blk = q4 * 4 + bb
                    for k in range(8):
                        S_.pe(lambda e, k=k, blk=blk, bb=bb, bank=bank: e.matmul(
                            bank[:, bb * 128:(bb + 1) * 128], lhsT=wfm[:, k, blk * 128:(blk + 1) * 128], rhs=uT[:, k, :],
                            start=(k == 0), stop=(k == 7)), r=[uT.b, wfm.b], w=[bank.b])
                S_.act(lambda e, q4=q4, bank=bank: e.copy(out=rawfm[:, q4 * 4:(q4 + 1) * 4, 3:131], in_=v3(bank[:, 0:512], 4)),
                       r=[bank.b], w=[rawfm.b])
                for bb in range(4):
                    blk = q4 * 4 + bb
                    eng = S_.dve
                    cw = convw[:, blk * 5:(blk + 1) * 5]
                    eng(lambda e, blk=blk, bb=bb, cw=cw: e.tensor_scalar(out=fmc[:, bb, :], in0=rawfm[:, blk, 0:128], scalar1=cw[:, 0:1],
                                                                        scalar2=cw[:, 4:5], op0=ALU.mult, op1=ALU.add),
                        r=[rawfm.b, convw.b], w=[fmc.b])
                    for k in range(1, 4):
                        eng(lambda e, blk=blk, bb=bb, cw=cw, k=k: e.scalar_tensor_tensor(out=fmc[:, bb, :], in0=rawfm[:, blk, k:k + 128],
                                                                                      scalar=cw[:, k:k + 1], in1=fmc[:, bb, :],
                                                                                      op0=ALU.mult, op1=ALU.add),
                            r=[rawfm.b, convw.b, fmc.b], w=[fmc.b])
                S_.act(lambda e, q4=q4: e.activation(out=fmb[:, q4 * 4:(q4 + 1) * 4, :], in_=fmc[:], func=AF.Silu), r=[fmc.b], w=[fmb.b])
            S_.pool(lambda e: e.tensor_copy(out=rawfm[:, :, 0:3], in_=rawfm[:, :, 128:131]), r=[rawfm.b], w=[rawfm.b])

            if marks is not None:
                marks.append((t, '---------- C: retention ----', len(S_.ops)))
            for h in range(2):
                S_.pe(lambda e, h=h: e.transpose(out=PTv[:, h, :], in_=qk0[:, 256 + h * 128:256 + (h + 1) * 128], identity=ident),
                      r=[qk0.b, cstb.b], w=[PT.b])
                S_.pe(lambda e, h=h: e.transpose(out=PTv[:, 2 + h, :], in_=kd[:, h * 128:(h + 1) * 128], identity=ident),
                      r=[kd.b, cstb.b], w=[PT.b])
            S_.act(lambda e: e.copy(out=qdT[0:64, :, 0, :], in_=PTv[0:64, 0:2, :]), r=[PT.b], w=[qdT.b])
            S_.act(lambda e: e.copy(out=qdT[64:128, :, 1, :], in_=PTv[64:128, 0:2, :]), r=[PT.b], w=[qdT.b])
            for h in range(2):
                S_.act(lambda e, h=h, t=t: e.copy(out=kc[h][:, t * 128:(t + 1) * 128], in_=PTv[:, 2 + h, :]), r=[PT.b], w=[kcb[h][t]])
            streamX = []
            S_.capture(streamX)
            S_.pool(lambda e: e.tensor_tensor(out=v3(rqg[:], 2), in0=v3(qk0[:, 0:128], 2), in1=bc_last(cst[:, 384:386], 64),
                                              op=ALU.mult), r=[qk0.b, cst.b], w=[rqg.b])
            S_.pool(lambda e: e.tensor_tensor(out=v3(rkg[:], 2), in0=v3(qk0[:, 128:256], 2), in1=bc_last(cst[:, 386:388], 64),
                                              op=ALU.mult), r=[qk0.b, cst.b], w=[rkg.b])
            S_.pe(lambda e: e.transpose(out=PTv[:, 0, :], in_=qk0[:, 0:128], identity=ident), r=[qk0.b, cstb.b], w=[PT.b])
            S_.pe(lambda e: e.transpose(out=PTv[:, 1, :], in_=qk0[:, 128:256], identity=ident), r=[qk0.b, cstb.b], w=[PT.b])
            S_.pe(lambda e: e.transpose(out=PTv[:, 2, :], in_=rqg[:], identity=ident), r=[rqg.b, cstb.b], w=[PT.b])
            S_.act(lambda e: e.copy(out=rTk[:], in_=PTv[:, 1, :]), r=[PT.b], w=[rTk.b])
            S_.act(lambda e: e.copy(out=rTm[0:64, 0:4:2, :], in_=PTv[0:64, 0:3:2, :]), r=[PT.b], w=[rTm.b])
            S_.act(lambda e: e.copy(out=rTm[64:128, 1:4:2, :], in_=PTv[64:128, 0:3:2, :]), r=[PT.b], w=[rTm.b])
            for h in range(2):
                S_.pe(lambda e, h=h: e.matmul(P[5][:, h * 128:(h + 1) * 128], lhsT=rTk[:], rhs=rTm[:, h, :],
                                              start=True, stop=True), r=[rTk.b, rTm.b], w=[P5a])
            S_.dve(lambda e: e.tensor_tensor(out=b1[:, 0:256], in0=P[5][:, 0:256], in1=cst[:, 128:384], op=ALU.mult),
                   r=[P5a, cst.b], w=[b1.b])
            for h in range(2):
                S_.pe(lambda e, h=h: e.matmul(P[5][:, 256 + h * 128:256 + (h + 1) * 128], lhsT=b1[:, h * 128:(h + 1) * 128],
                                              rhs=rv[:, h * 128:(h + 1) * 128], start=True, stop=False), r=[b1.b, rv.b], w=[P5b])
                S_.pe(lambda e, h=h: e.matmul(P[5][:, 256 + h * 128:256 + (h + 1) * 128], lhsT=rTm[:, 2 + h, :],
                                              rhs=rstb[:], start=False, stop=True), r=[rTm.b, rstb.b], w=[P5b])
            for h in range(2):
                S_.pe(lambda e, h=h: e.matmul(P[6][:, h * 128:(h + 1) * 128], lhsT=rkg[:],
                                              rhs=rv[:, h * 128:(h + 1) * 128], start=True, stop=True),
                      r=[rkg.b, rv.b], w=[P6a])
            for h in range(2):
                ps = slice(h * 64, (h + 1) * 64)
                S_.dve(lambda e, h=h, ps=ps: e.scalar_tensor_tensor(out=rst[ps, :], in0=rst[ps, :], scalar=cst[ps, 388:389],
                                                                    in1=P[6][ps, h * 128:(h + 1) * 128],
                                                                    op0=ALU.mult, op1=ALU.add), r=[rst.b, cst.b, P6a], w=[rst.b])
            S_.act(lambda e: e.copy(out=rstb[:], in_=rst[:]), r=[rst.b], w=[rstb.b])
            S_.act(lambda e: e.activation(out=f1[:, 0:256], in_=P[5][:, 256:512], func=AF.Square, scale=1.0 / math.sqrt(128.0)),
                   r=[P5b], w=[f1.b])
            S_.dve(lambda e: e.tensor_reduce(out=stat[:, 2:4], in_=v3(f1[:, 0:256], 2), axis=AX.X, op=ALU.add),
                   r=[f1.b], w=[stat.b])
            S_.dve(lambda e: e.tensor_scalar(out=stat[:, 2:4], in0=stat[:, 2:4], scalar1=EPS, scalar2=None, op0=ALU.add),
                   r=[stat.b], w=[stat.b])
            S_.act(lambda e: e.activation(out=stat[:, 2:4], in_=stat[:, 2:4], func=AF.Sqrt), r=[stat.b], w=[stat.b])
            S_.dve(lambda e: e.reciprocal(out=stat[:, 2:4], in_=stat[:, 2:4]), r=[stat.b], w=[stat.b])
            S_.dve(lambda e: e.tensor_tensor(out=v3(f2[:, 0:256], 2), in0=v3(P[5][:, 256:512], 2), in1=bc_last(stat[:, 2:4], 128),
                                             op=ALU.mult), r=[P5b, stat.b], w=[f2.b])
            S_.pool(lambda e: e.tensor_tensor(out=mix[:, 0:256], in0=f2[:, 0:256], in1=zs[:, 0:256], op=ALU.mult),
                    r=[f2.b, zs.b], w=[mix.b])

            if marks is not None:
                marks.append((t, '---------- E: SSD ----------', len(S_.ops)))
            for i in range(6):
                S_.pe(lambda e, i=i: e.transpose(out=PTv[:, i, :], in_=fmb[:, i, :], identity=ident), r=[fmb.b, cstb.b], w=[PT.b])
            S_.act(lambda e: e.copy(out=b2[:, 0:768], in_=PT[:, 0:768]), r=[PT.b], w=[b2.b])
            S_.pool(lambda e: e.tensor_tensor(out=v3(b3[:, 0:512], 8), in0=v3(b2[:, 0:512], 8), in1=bc_last(g6b[:, 0:8], 64),
                                              op=ALU.mult), r=[b2.b, g6b.b], w=[b3.b])
            S_.dve(lambda e: e.tensor_copy(out=hl[:, 0, 0:8], in_=dA[:, 0:8]), r=[dA.b], w=[hl.b])
            S_.dve(lambda e: e.tensor_tensor(out=hl[:, 1, 0:8], in0=dA[:, 0:8], in1=hl[:, 0, 0:8], op=ALU.subtract), r=[dA.b, hl.b], w=[hl.b])
            for g in range(2):
                S_.pool(lambda e, g=g: e.tensor_tensor(
                    out=gbb[:], in0=maskTb.unsqueeze(1).unsqueeze(1).broadcast_to([128, 2, 4, 128]),
                    in1=hl[:, :, g * 4:(g + 1) * 4].unsqueeze(3).broadcast_to([128, 2, 4, 128]), op=ALU.mult),
                    r=[cstb.b, hl.b], w=[gbb.b])
                for q in range(2):
                    S_.pe(lambda e, g=g, q=q: e.matmul(P[5 + g][:, 0:512], lhsT=Lstb, rhs=gbb[:, q, :, :].rearrange("p a b -> p (a b)"),
                                                       start=(q == 0), stop=(q == 1)), r=[cstb.b, gbb.b], w=[R["P%da" % (5 + g)], R["P%db" % (5 + g)]])
            for g in range(2):
                S_.act(lambda e, g=g: e.activation(out=b1[:, g * 512:(g + 1) * 512], in_=P[5 + g][:, 0:512], func=AF.Exp),
                       r=[R["P%da" % (5 + g)], R["P%db" % (5 + g)]], w=[b1.b])
                S_.act(lambda e, g=g: e.activation(out=stat[:, 8 + g * 4:12 + g * 4], in_=v3(P[5 + g][:, 0:512], 4)[:, :, 127],
                                                   func=AF.Exp), r=[R["P%da" % (5 + g)], R["P%db" % (5 + g)]], w=[stat.b])
            for q in range(2):
                S_.pe(lambda e, q=q: e.matmul(P[5][:, 0:8], lhsT=maskTb, rhs=hl[:, q, 0:8], start=(q == 0), stop=(q == 1)), r=[cstb.b, hl.b], w=[P5a, P5b])
            for q in range(2):
                S_.pe(lambda e, q=q: e.matmul(P[5][:, 8:16], lhsT=onesb, rhs=hl[:, q, 0:8], start=(q == 0), stop=(q == 1)), r=[cstb.b, hl.b], w=[P5a, P5b])
            S_.act(lambda e: e.activation(out=stat[:, 16:32], in_=P[5][:, 0:16], func=AF.Exp), r=[P5a, P5b], w=[stat.b])
            for g in range(2):
                S_.pe(lambda e, g=g: e.matmul(P[6][:, g * 128:(g + 1) * 128], lhsT=fmb[:, 4 + g, :], rhs=fmb[:, 6 + g, :],
                                              start=True, stop=True), r=[fmb.b], w=[P6a, P6b])
            S_.dve(lambda e: e.tensor_tensor(out=v3(f1[:, 0:256], 2), in0=v3(P[6][:, 0:256], 2), in1=bc_mid(maskT, 2), op=ALU.mult),
                   r=[P6a, P6b, cst.b], w=[f1.b])
            for g in range(2):
                S_.pool(lambda e, g=g: e.tensor_tensor(out=v3(b1[:, g * 512:(g + 1) * 512], 4), in0=v3(b1[:, g * 512:(g + 1) * 512], 4),
                                                       in1=bc_mid(f1[:, g * 128:(g + 1) * 128], 4), op=ALU.mult),
                        r=[b1.b, f1.b], w=[b1.b])
            S_.pool(lambda e: e.tensor_tensor(out=v3(b3[:, 512:1024], 8), in0=v3(b3[:, 0:512], 8), in1=bc_last(stat[:, 8:16], 64),
                                              op=ALU.mult), r=[b3.b, stat.b], w=[b3.b])
            for hh in range(8):
                S_.pe(lambda e, hh=hh: e.matmul(P[5][:, hh * 64:(hh + 1) * 64], lhsT=b1[:, hh * 128:(hh + 1) * 128],
                                                rhs=b3[:, hh * 64:(hh + 1) * 64], start=True, stop=True),
                      r=[b1.b, b3.b], w=[P5a, P5b])
            for g in range(2):
                S_.pe(lambda e, g=g: e.matmul(P[6][:, g * 256:(g + 1) * 256], lhsT=fmb[:, 6 + g, :], rhs=sstb[:, g * 256:(g + 1) * 256],
                                              start=True, stop=True), r=[fmb.b, sstb.b], w=[P6a, P6b])
            S_.dve(lambda e: e.tensor_tensor(out=v3(f2[:], 8), in0=v3(P[6][:, 0:512], 8), in1=bc_last(stat[:, 16:24], 64), op=ALU.mult),
                   r=[P6a, P6b, stat.b], w=[f2.b])
            S_.dve(lambda e: e.tensor_tensor(out=f2[:], in0=P[5][:, 0:512], in1=f2[:], op=ALU.add), r=[P5a, P5b, f2.b], w=[f2.b])
            S_.pool(lambda e: e.tensor_tensor(out=v3(f3[:], 8), in0=v3(b2[:, 0:512], 8), in1=bc_last(par[:, PAR_DS:PAR_DS + 8], 64),
                                              op=ALU.mult), r=[b2.b, par.b], w=[f3.b])
            S_.pool(lambda e: e.tensor_tensor(out=f2[:], in0=f2[:], in1=f3[:], op=ALU.add), r=[f2.b, f3.b], w=[f2.b])
            S_.pool(lambda e: e.tensor_tensor(out=f2[:], in0=f2[:], in1=zs[:, 512:1024], op=ALU.mult), r=[f2.b, zs.b], w=[f2.b])
            for g in range(2):
                S_.pe(lambda e, g=g: e.matmul(P[6][:, g * 256:(g + 1) * 256], lhsT=b2[:, 512 + g * 128:512 + (g + 1) * 128],
                                              rhs=b3[:, 512 + g * 256:512 + (g + 1) * 256], start=True, stop=True),
                      r=[b2.b, b3.b], w=[P6a, P6b])
            S_.pool(lambda e: e.tensor_tensor(out=v3(sst[:], 8), in0=v3(sst[:], 8), in1=bc_last(stat[:, 24:32], 64), op=ALU.mult),
                    r=[sst.b, stat.b], w=[sst.b])
            S_.dve(lambda e: e.tensor_tensor(out=sst[:], in0=sst[:], in1=P[6][:, 0:512], op=ALU.add), r=[sst.b, P6a, P6b], w=[sst.b])
            S_.act(lambda e: e.copy(out=sstb[:], in_=sst[:]), r=[sst.b], w=[sstb.b])
            S_.act(lambda e: e.activation(out=f3[:], in_=f2[:], func=AF.Square, scale=1.0 / math.sqrt(512.0), accum_out=stat[:, 4:5]),
                   r=[f2.b], w=[f3.b, stat.b])
            S_.dve(lambda e: e.tensor_scalar(out=stat[:, 4:5], in0=stat[:, 4:5], scalar1=EPS, scalar2=None, op0=ALU.add),
                   r=[stat.b], w=[stat.b])
            S_.act(lambda e: e.activation(out=stat[:, 4:5], in_=stat[:, 4:5], func=AF.Sqrt), r=[stat.b], w=[stat.b])
            S_.dve(lambda e: e.reciprocal(out=stat[:, 4:5], in_=stat[:, 4:5]), r=[stat.b], w=[stat.b])
            S_.dve(lambda e: e.scalar_tensor_tensor(out=mix[:, 512:768], in0=f2[:, 0:256], scalar=stat[:, 4:5],
                                                    in1=par[:, PAR_SNW:PAR_SNW + 256], op0=ALU.mult, op1=ALU.mult),
                   r=[f2.b, stat.b, par.b], w=[mix.b])

            if marks is not None:
                marks.append((t, '---------- F: mLSTM --------', len(S_.ops)))
            for h in range(2):
                S_.pe(lambda e, h=h: e.transpose(out=PTv[:, h, :], in_=fmb[:, 10 + h, :], identity=ident), r=[fmb.b, cstb.b], w=[PT.b])
            S_.act(lambda e: e.copy(out=b2[:, 768:1024], in_=PT[:, 0:256]), r=[PT.b], w=[b2.b])
            S_.dve(lambda e: e.tensor_copy(out=hl[:, 0, 8:10], in_=g6b[:, 8:10]), r=[g6b.b], w=[hl.b])
            S_.dve(lambda e: e.tensor_tensor(out=hl[:, 1, 8:10], in0=g6b[:, 8:10], in1=hl[:, 0, 8:10], op=ALU.subtract), r=[g6b.b, hl.b], w=[hl.b])
            S_.pool(lambda e: e.tensor_tensor(
                out=gbb[:, :, 0:2, :], in0=maskTb.unsqueeze(1).unsqueeze(1).broadcast_to([128, 2, 2, 128]),
                in1=hl[:, :, 8:10].unsqueeze(3).broadcast_to([128, 2, 2, 128]), op=ALU.mult), r=[cstb.b, hl.b], w=[gbb.b])
            for q in range(2):
                S_.pe(lambda e, q=q: e.matmul(P[5][:, 0:256], lhsT=Lstb, rhs=gbb[:, q, 0:2, :].rearrange("p a b -> p (a b)"),
                                              start=(q == 0), stop=(q == 1)), r=[cstb.b, gbb.b], w=[P5a])
            for h in range(2):
                S_.act(lambda e, h=h: e.activation(out=f1[:, h * 128:(h + 1) * 128], in_=P[5][:, h * 128:(h + 1) * 128], func=AF.Exp,
                                                   bias=g6[:, 10 + h:11 + h]), r=[P5a, g6.b], w=[f1.b])
            S_.dve(lambda e: e.scalar_tensor_tensor(out=v3(f1[:, 0:256], 2), in0=v3(f1[:, 0:256], 2), scalar=1.0 / math.sqrt(128.0),
                                                     in1=bc_mid(maskT, 2), op0=ALU.mult, op1=ALU.mult), r=[f1.b, cst.b], w=[f1.b])
            for h in range(2):
                S_.pe(lambda e, h=h: e.matmul(P[5][:, 256 + h * 128:256 + (h + 1) * 128], lhsT=fmb[:, 10 + h, :], rhs=fmb[:, 8 + h, :],
                                              start=True, stop=True), r=[fmb.b], w=[P5b])
            S_.dve(lambda e: e.tensor_tensor(out=b1[:, 0:256], in0=P[5][:, 256:512], in1=f1[:, 0:256], op=ALU.mult),
                   r=[P5b, f1.b], w=[b1.b])
            for q in range(2):
                S_.pe(lambda e, q=q: e.matmul(P[6][:, 0:2], lhsT=maskTb, rhs=hl[:, q, 8:10], start=(q == 0), stop=(q == 1)), r=[cstb.b, hl.b], w=[P6a])
            for q in range(2):
                S_.pe(lambda e, q=q: e.matmul(P[6][:, 2:4], lhsT=onesb, rhs=hl[:, q, 8:10], start=(q == 0), stop=(q == 1)), r=[cstb.b, hl.b], w=[P6a])
            S_.act(lambda e: e.activation(out=stat[:, 8:12], in_=P[6][:, 0:4], func=AF.Exp), r=[P6a], w=[stat.b])
            S_.dve(lambda e: e.tensor_tensor(out=stat[:, 12:14], in0=g6[:, 10:12], in1=P[6][:, 0:2], op=ALU.subtract),
                   r=[g6.b, P6a], w=[stat.b])
            S_.dve(lambda e: e.tensor_tensor(out=stat[:, 12:14], in0=stat[:, 12:14], in1=P[6][:, 2:4], op=ALU.add),
                   r=[stat.b, P6a], w=[stat.b])
            S_.act(lambda e: e.activation(out=stat[:, 12:14], in_=stat[:, 12:14], func=AF.Exp), r=[stat.b], w=[stat.b])
            P5bv = P[5][:, 0:260].rearrange("p (a b) -> p a b", a=2)
            P6bv = P[6][:, 0:260].rearrange("p (a b) -> p a b", a=2)
            for h in range(2):
                S_.pe(lambda e, h=h: e.matmul(P5bv[:, h, :], lhsT=b1[:, h * 128:(h + 1) * 128], rhs=mlv[:, h, :], start=True, stop=True),
                      r=[b1.b, mlv.b], w=[P5a, P5b])
            for h in range(2):
                S_.pe(lambda e, h=h: e.matmul(P6bv[:, h, :], lhsT=fmb[:, 8 + h, :], rhs=mstb[:, h, :], start=True, stop=True),
                      r=[fmb.b, mstb.b], w=[P6a, P6b])
            f1v = f1[:, 0:260].rearrange("p (a b) -> p a b", a=2)
            S_.act(lambda e: e.copy(out=f1v, in_=P5bv), r=[P5a, P5b], w=[f1.b])
            for h in range(2):
                S_.dve(lambda e, h=h: e.scalar_tensor_tensor(out=f1v[:, h, :], in0=P6bv[:, h, :], scalar=stat[:, 8 + h:9 + h],
                                                             in1=f1v[:, h, :], op0=ALU.mult, op1=ALU.add),
                       r=[P6a, P6b, stat.b, f1.b], w=[f1.b])
            S_.dve(lambda e: e.scalar_tensor_tensor(out=v3(b3[:, 0:260], 2), in0=mlv[:], scalar=1.0 / math.sqrt(128.0),
                                                     in1=bc_last(stat[:, 12:14], 130), op0=ALU.mult, op1=ALU.mult),
                    r=[mlv.b, stat.b], w=[b3.b])
            for h in range(2):
                S_.pe(lambda e, h=h: e.matmul(P6bv[:, h, :], lhsT=b2[:, 768 + h * 128:768 + (h + 1) * 128],
                                              rhs=b3[:, h * 130:(h + 1) * 130], start=True, stop=True), r=[b2.b, b3.b], w=[P6a, P6b])
            S_.pool(lambda e: e.tensor_tensor(out=mst[:], in0=mst[:], in1=bc_last(stat[:, 10:12], 130), op=ALU.mult),
                    r=[mst.b, stat.b], w=[mst.b])
            S_.dve(lambda e: e.tensor_tensor(out=mst[:], in0=mst[:], in1=P6bv, op=ALU.add), r=[mst.b, P6a, P6b], w=[mst.b])
            S_.act(lambda e: e.copy(out=mstb[:], in_=mst[:]), r=[mst.b], w=[mstb.b])
            S_.act(lambda e: e.activation(out=stat[:, 14:16], in_=f1v[:, :, 128], func=AF.Abs), r=[f1.b], w=[stat.b])
            S_.dve(lambda e: e.tensor_scalar(out=stat[:, 14:16], in0=stat[:, 14:16], scalar1=1.0, scalar2=None, op0=ALU.max),
                   r=[stat.b], w=[stat.b])
            S_.dve(lambda e: e.reciprocal(out=stat[:, 14:16], in_=stat[:, 14:16]), r=[stat.b], w=[stat.b])
            S_.dve(lambda e: e.tensor_tensor(out=v3(f2[:, 0:256], 2), in0=f1v[:, :, 0:128], in1=bc_last(stat[:, 14:16], 128), op=ALU.mult),
                   r=[f1.b, stat.b], w=[f2.b])
            S_.pool(lambda e: e.tensor_tensor(out=f2[:, 0:256], in0=f2[:, 0:256], in1=sgo[:], op=ALU.mult), r=[f2.b, sgo.b], w=[f2.b])
            S_.dve(lambda e: e.tensor_reduce(out=stat[:, 5:7], in_=v3(f2[:, 0:256], 2), axis=AX.X, op=ALU.add), r=[f2.b], w=[stat.b])
            S_.dve(lambda e: e.tensor_scalar(out=stat[:, 5:7], in0=stat[:, 5:7], scalar1=-1.0 / 128.0, scalar2=None, op0=ALU.mult),
                   r=[stat.b], w=[stat.b])
            S_.dve(lambda e: e.tensor_tensor(out=v3(f2[:, 0:256], 2), in0=v3(f2[:, 0:256], 2), in1=bc_last(stat[:, 5:7], 128), op=ALU.add),
                   r=[f2.b, stat.b], w=[f2.b])
            S_.act(lambda e: e.activation(out=f3[:, 0:256], in_=f2[:, 0:256], func=AF.Square, scale=1.0 / math.sqrt(128.0)),
                   r=[f2.b], w=[f3.b])
            S_.dve(lambda e: e.tensor_reduce(out=stat[:, 5:7], in_=v3(f3[:, 0:256], 2), axis=AX.X, op=ALU.add), r=[f3.b], w=[stat.b])
            S_.dve(lambda e: e.tensor_scalar(out=stat[:, 5:7], in0=stat[:, 5:7], scalar1=EPS, scalar2=None, op0=ALU.add),
                   r=[stat.b], w=[stat.b])
            S_.act(lambda e: e.activation(out=stat[:, 5:7], in_=stat[:, 5:7], func=AF.Sqrt), r=[stat.b], w=[stat.b])
            S_.dve(lambda e: e.reciprocal(out=stat[:, 5:7], in_=stat[:, 5:7]), r=[stat.b], w=[stat.b])
            S_.dve(lambda e: e.tensor_tensor(out=v3(f2[:, 0:256], 2), in0=v3(f2[:, 0:256], 2), in1=bc_last(stat[:, 5:7], 128), op=ALU.mult),
                   r=[f2.b, stat.b], w=[f2.b])
            S_.pool(lambda e: e.tensor_tensor(out=f2[:, 0:256], in0=f2[:, 0:256], in1=par[:, PAR_MNW:PAR_MNW + 256], op=ALU.mult),
                    r=[f2.b, par.b], w=[f2.b])
            S_.pool(lambda e: e.tensor_tensor(out=mix[:, 768:1024], in0=f2[:, 0:256], in1=zs[:, 1024:1280], op=ALU.mult),
                    r=[f2.b, zs.b], w=[mix.b])

            if marks is not None:
                marks.append((t, '---------- D: differential a', len(S_.ops)))
            streamY = []
            S_.capture(streamY)
            nblk = t + 1
            for h in range(2):
                accs = []
                for tt in range(2):
                    ps = slice(tt * 64, (tt + 1) * 64)
                    acc = P[2 + (acc_i[0] % 2)]
                    acc_i[0] += 1
                    accs.append(acc)
                    ngrp = (nblk + 3) // 4
                    for gi in range(ngrp):
                        j0 = gi * 4
                        nb = min(4, nblk - j0)
                        scb = P[4] if (gi % 2 == 0) else P[0]
                        scw = [scb.b]
                        pb = pbf[gi % 2]
                        for jj in range(nb):
                            j = j0 + jj
                            S_.pe(lambda e, jj=jj, j=j, tt=tt, h=h, scb=scb: e.matmul(
                                scb[:, jj * 128:(jj + 1) * 128], lhsT=kc[h][:, j * 128:(j + 1) * 128], rhs=qdT[:, h, tt, :],
                                start=True, stop=True), r=[kcb[h][j], qdT.b], w=scw)
                        S_.act(lambda e, nb=nb, scb=scb, pb=pb: e.activation(out=pb[:, 0:nb, :], in_=v3(scb[:, 0:nb * 128], nb),
                                                                            func=AF.Exp, scale=0.125), r=scw, w=[pb.b])
                        if j0 + nb - 1 == t:
                            jd = nb - 1
                            S_.pool(lambda e, jd=jd, pb=pb: e.tensor_tensor(out=pb[:, jd, :], in0=pb[:, jd, :], in1=maskTb, op=ALU.mult),
                                    r=[pb.b, cstb.b], w=[pb.b])
                        for jj in range(nb):
                            j = j0 + jj
                            S_.pe(lambda e, jj=jj, j=j, h=h, acc=acc, pb=pb, nblk=nblk: e.matmul(
                                acc[:, 0:130], lhsT=pb[:, jj, :], rhs=vc[h][:, j, :], start=(j == 0), stop=(j == nblk - 1)),
                                r=[pb.b, vcb[h][j]], w=[acc.b])
                a1, a2 = accs
                c0 = h * 2
                S_.dve(lambda e, a1=a1, c0=c0: e.reciprocal(out=statd[:, c0:c0 + 1], in_=a1[:, 128:129]), r=[a1.b], w=[statd.b])
                S_.dve(lambda e, a2=a2, c0=c0: e.reciprocal(out=statd[:, c0 + 1:c0 + 2], in_=a2[:, 128:129]), r=[a2.b], w=[statd.b])
                S_.dve(lambda e, c0=c0: e.tensor_tensor(out=statd[:, c0 + 1:c0 + 2], in0=statd[:, c0 + 1:c0 + 2], in1=small[:, 20:21], op=ALU.mult),
                       r=[statd.b, small.b], w=[statd.b])
                S_.act(lambda e, a1=a1, c0=c0, h=h: e.activation(out=fd[:, h * 128:(h + 1) * 128], in_=a1[:, 0:128], func=AF.Copy,
                                                                scale=statd[:, c0:c0 + 1]), r=[a1.b, statd.b], w=[fd.b])
                S_.dve(lambda e, a2=a2, c0=c0, h=h: e.scalar_tensor_tensor(out=fd[:, h * 128:(h + 1) * 128], in0=a2[:, 0:128],
                                                                          scalar=statd[:, c0 + 1:c0 + 2], in1=fd[:, h * 128:(h + 1) * 128],
                                                                          op0=ALU.mult, op1=ALU.add), r=[a2.b, statd.b, fd.b], w=[fd.b])
            S_.act(lambda e: e.activation(out=fd[:, 256:512], in_=fd[:, 0:256], func=AF.Square, scale=1.0 / math.sqrt(128.0)),
                   r=[fd.b], w=[fd.b])
            S_.dve(lambda e: e.tensor_reduce(out=statd[:, 4:6], in_=v3(fd[:, 256:512], 2), axis=AX.X, op=ALU.add), r=[fd.b], w=[statd.b])
            S_.dve(lambda e: e.tensor_scalar(out=statd[:, 4:6], in0=statd[:, 4:6], scalar1=EPS, scalar2=None, op0=ALU.add),
                   r=[statd.b], w=[statd.b])
            S_.act(lambda e: e.activation(out=statd[:, 4:6], in_=statd[:, 4:6], func=AF.Sqrt), r=[statd.b], w=[statd.b])
            S_.dve(lambda e: e.reciprocal(out=statd[:, 4:6], in_=statd[:, 4:6]), r=[statd.b], w=[statd.b])
            S_.dve(lambda e: e.tensor_tensor(out=v3(fd[:, 0:256], 2), in0=v3(fd[:, 0:256], 2), in1=bc_last(statd[:, 4:6], 128), op=ALU.mult),
                   r=[fd.b, statd.b], w=[fd.b])
            S_.pool(lambda e: e.tensor_tensor(out=v3(fd[:, 0:256], 2), in0=v3(fd[:, 0:256], 2), in1=bc_mid(par[:, PAR_DNW:PAR_DNW + 128], 2),
                                              op=ALU.mult), r=[fd.b, par.b], w=[fd.b])
            S_.pool(lambda e: e.tensor_tensor(out=mix[:, 256:512], in0=fd[:, 0:256], in1=zs[:, 256:512], op=ALU.mult),
                    r=[fd.b, zs.b], w=[mix.b])

            if marks is not None:
                marks.append((t, '---------- G: out-proj -----', len(S_.ops)))
            S_.capture(None)
            S_.merge(streamX, streamY)
            for k in range(8):
                S_.pe(lambda e, k=k: e.transpose(out=PTv[:, k, :], in_=mix[:, k * 128:(k + 1) * 128], identity=ident),
                      r=[mix.b, cstb.b], w=[PT.b])
            S_.act(lambda e: e.copy(out=mixT[:], in_=PTv), r=[PT.b], w=[mixT.b])
            for half in range(2):
                bank = P[half]
                for k in range(8):
                    S_.pe(lambda e, k=k, half=half, bank=bank: e.matmul(bank[:, 0:512], lhsT=mixT[:, k, :],
                                                                        rhs=wout[:, k, half * 512:(half + 1) * 512],
                                                                        start=(k == 0), stop=(k == 7)), r=[mixT.b, wout.b], w=[bank.b])
                if half == 0:
                    S_.act(lambda e, bank=bank: e.copy(out=f1[:], in_=bank[:, 0:512]), r=[bank.b], w=[f1.b])
                else:
                    S_.dve(lambda e, bank=bank: e.tensor_copy(out=f2[:], in_=bank[:, 0:512]), r=[bank.b], w=[f2.b])
            ob = Buf("outd")
            obs.append(ob)
            S_.dma("sp", lambda e, tok=tok: e.dma_start(out=out_d[tok, 0:512], in_=f1[:]), r=[f1.b], w=[ob])
            ob = Buf("outd")
            obs.append(ob)
            S_.dma("sp", lambda e, tok=tok: e.dma_start(out=out_d[tok, 512:1024], in_=f2[:]), r=[f2.b], w=[ob])
            if debug:
                for q in range(2):
                    S_.act(lambda e, q=q: e.copy(out=f3[:], in_=mix[:, q * 512:(q + 1) * 512]), r=[mix.b], w=[f3.b])
                    ob = Buf("dbgd")
                    obs.append(ob)
                    S_.dma("sp", lambda e, tok=tok, q=q: e.dma_start(out=dbg_d[tok, q * 512:(q + 1) * 512], in_=f3[:]), r=[f3.b], w=[ob])
        if trunc is not None:
            S_.ops = S_.ops[:trunc]
        else:
            S_.add("sp", None, reads=obs)
        S_.emit(st)
    return nc


def build_final(ntok, nparts):
    nc = bass.Bass("TRN2", target_bir_lowering=False)
    dr = lambda n, shp, kind="ExternalInput": nc.dram_tensor(n, shp, F32, kind=kind).ap()
    ins = [dr("p%d" % i, [ntok, D_MODEL]) for i in range(nparts)]
    w_d = dr("w", [1, D_MODEL])
    out_d = dr("out", [ntok, D_MODEL], kind="ExternalOutput")
    with ExitStack() as st:
        S_ = Sched(nc)
        sb = lambda n, shp, dt=F32: T(st, nc, n, shp, dt)
        wt = sb("wt", [128, D_MODEL])
        xs = [[sb("x%d_%d" % (i, q), [128, D_MODEL]) for i in range(nparts)] for q in range(2)]
        junk = sb("junk", [128, D_MODEL])
        stat = sb("stat", [128, 4])
        S_.dma("sp", lambda e: e.dma_start(out=wt[:], in_=w_d[0:1, :].broadcast_to([128, D_MODEL])), w=[wt.b])
        ob = Buf("o")
        for t in range(ntok // 128):
            tok = slice(t * 128, (t + 1) * 128)
            xx = xs[t % 2]
            for i in range(nparts):
                S_.dma("sp", lambda e, i=i, xx=xx, tok=tok: e.dma_start(out=xx[i][:], in_=ins[i][tok, :]), w=[xx[i].b])
            for i in range(1, nparts):
                eng = S_.pool if i % 2 else S_.dve
                eng(lambda e, i=i, xx=xx: e.tensor_tensor(out=xx[0][:], in0=xx[0][:], in1=xx[i][:], op=ALU.add),
                    r=[xx[0].b, xx[i].b], w=[xx[0].b])
            S_.act(lambda e, xx=xx: e.activation(out=junk[:], in_=xx[0][:], func=AF.Square, scale=1.0 / 32.0, accum_out=stat[:, 0:1]),
                   r=[xx[0].b], w=[junk.b, stat.b])
            S_.dve(lambda e: e.tensor_scalar(out=stat[:, 0:1], in0=stat[:, 0:1], scalar1=EPS, scalar2=None, op0=ALU.add),
                   r=[stat.b], w=[stat.b])
            S_.act(lambda e: e.activation(out=stat[:, 1:2], in_=stat[:, 0:1], func=AF.Sqrt), r=[stat.b], w=[stat.b])
            S_.dve(lambda e: e.reciprocal(out=stat[:, 1:2], in_=stat[:, 1:2]), r=[stat.b], w=[stat.b])
            S_.dve(lambda e, xx=xx: e.scalar_tensor_tensor(out=xx[1][:], in0=xx[0][:], scalar=stat[:, 1:2], in1=wt[:],
                                                           op0=ALU.mult, op1=ALU.mult), r=[xx[0].b, stat.b, wt.b], w=[xx[1].b])
            S_.dma("sp", lambda e, xx=xx, tok=tok: e.dma_start(out=out_d[tok, :], in_=xx[1][:]), r=[xx[1].b], w=[ob])
        S_.add("sp", None, reads=[ob])
        S_.emit(st)
    return nc


def layer_inputs(l, j, S, p):
    tm, fm, orow = _col_index(j)
    hs = (2 * j, 2 * j + 1)
    gs = (j, 1 - j)
    w_in = p["w_in"][l]
    d = {}
    d["wtm"] = np.ascontiguousarray(w_in[:, tm])
    d["wfm"] = np.ascontiguousarray(w_in[:, fm])
    d["wout"] = np.ascontiguousarray(p["w_out"][l][orow, :])
    d["rope"] = _rope(S)
    d["cst"], d["cst2"] = _consts(j)
    d["nwfm"] = np.ascontiguousarray(p["norm_w"][l].reshape(8, 128).T)
    par = np.zeros((1, NPAR), np.float32)
    par[0, 0:128] = p["diff_norm_w"][l]
    par[0, 128:384] = p["ssd_norm_w"][l][j * 256:(j + 1) * 256]
    par[0, 384:640] = p["mlstm_norm_w"][l][2 * j * 128:(2 * j + 2) * 128]
    gi = np.concatenate([g * 4 + np.arange(4) for g in gs])
    par[0, 640:648] = p["ssd_dt_bias"][l][gi]
    par[0, 648:650] = p["mlstm_gate_b"][l][4 + np.array(hs)]
    par[0, 650:652] = p["mlstm_gate_b"][l][np.array(hs)]
    par[0, 652:660] = p["ssd_a_log"][l][gi]
    par[0, 660:668] = p["ssd_d"][l][gi]
    par[0, 668:924] = p["diff_lambda"][l].reshape(-1)
    d["par"] = par
    xbc_ch = np.concatenate([g * 256 + np.arange(256) for g in gs] + [512 + g * 128 + np.arange(128) for g in gs]
                            + [768 + g * 128 + np.arange(128) for g in gs])
    qk_ch = np.concatenate([h * 128 + np.arange(128) for h in hs] + [512 + h * 128 + np.arange(128) for h in hs])
    cw = np.concatenate([p["ssd_conv_w"][l][:, xbc_ch], p["mlstm_conv_w"][l][:, qk_ch]], axis=1)
    cb = np.concatenate([p["ssd_conv_b"][l][xbc_ch], p["mlstm_conv_b"][l][qk_ch]])[None, :]
    c5 = np.concatenate([cw, cb], axis=0)
    d["convw"] = np.ascontiguousarray(c5.reshape(5, 12, 128).transpose(2, 1, 0).reshape(128, 60))
    return d


_PROG_CACHE = {}


def _prog(key, fn):
    if key not in _PROG_CACHE:
        _PROG_CACHE[key] = fn()
    return _PROG_CACHE[key]


def kernel(x, norm_w, w_in, w_out, diff_lambda, diff_norm_w, ssd_conv_w, ssd_conv_b, ssd_dt_bias,
           ssd_a_log, ssd_d, ssd_norm_w, mlstm_conv_w, mlstm_conv_b, mlstm_gate_b, mlstm_norm_w, final_norm_w):
    p = dict(norm_w=norm_w, w_in=w_in, w_out=w_out, diff_lambda=diff_lambda, diff_norm_w=diff_norm_w,
             ssd_conv_w=ssd_conv_w, ssd_conv_b=ssd_conv_b, ssd_dt_bias=ssd_dt_bias, ssd_a_log=ssd_a_log,
             ssd_d=ssd_d, ssd_norm_w=ssd_norm_w, mlstm_conv_w=mlstm_conv_w, mlstm_conv_b=mlstm_conv_b,
             mlstm_gate_b=mlstm_gate_b, mlstm_norm_w=mlstm_norm_w)
    p = {k: np.asarray(v, np.float32) for k, v in p.items()}
    x = np.asarray(x, np.float32)
    Bn, S, _ = x.shape
    depth = w_in.shape[0]
    ncores = 2 * Bn
    partials = []
    for l in range(depth):
        lam_init = 0.8 - 0.6 * math.exp(-0.3 * l)
        nprev = 2 * l
        nc = build_layer(S, nprev, lam_init)
        in_maps = []
        for c in range(ncores):
            b, j = c // 2, c % 2
            d = layer_inputs(l, j, S, p)
            d["x"] = np.ascontiguousarray(x[b])
            i = 0
            for pl in partials:
                for jj in range(2):
                    d["prev%d" % i] = pl[2 * b + jj]
                    i += 1
            in_maps.append(d)
        res = run_bass_kernel_spmd(nc, in_maps, core_ids=list(range(ncores)))
        partials.append([res.results[c]["out"] for c in range(ncores)])
    ntok = Bn * S // ncores
    nparts = 1 + 2 * depth
    nc = build_final(ntok, nparts)
    in_maps = []
    for c in range(ncores):
        b = (c * ntok) // S
        o = (c * ntok) % S
        d = {"p0": np.ascontiguousarray(x[b, o:o + ntok]), "w": np.asarray(final_norm_w, np.float32).reshape(1, -1)}
        i = 1
        for pl in partials:
            for jj in range(2):
                d["p%d" % i] = np.ascontiguousarray(pl[2 * b + jj][o:o + ntok])
                i += 1
        in_maps.append(d)
    res = run_bass_kernel_spmd(nc, in_maps, core_ids=list(range(ncores)))
    out = np.concatenate([res.results[c]["out"] for c in range(ncores)], axis=0).reshape(Bn, S, D_MODEL)
    return out.astype(np.float32)
```
